# Optimizing a Trainium2 kernel written in Bass

```python
import jax, jax.numpy as jnp
from jax import lax
import numpy as np

D_MODEL = 1024
BATCH = 8
SEQ = 4096
DEPTH = 2

HEAD_DIM_A = 64
WIDTH_A = D_MODEL // 2
N_HEADS_A = WIDTH_A // HEAD_DIM_A
HEAD_DIM_B = 64
WIDTH_B = D_MODEL // 2
N_HEADS_B = WIDTH_B // HEAD_DIM_B
CONV_K = 5
CHUNK = 64
N_DIR = 2
N_GROUPS = 4
EXPERTS_PER_GROUP = 8
N_EXPERTS = N_GROUPS * EXPERTS_PER_GROUP
TOP_K = 2
D_EXPERT = D_MODEL // 4
N_MOD = 6
EPS = 1e-6
NEG_INF = -1e30

SPLIT_WIDTHS = (3 * WIDTH_A, WIDTH_A, 2 * N_DIR * N_HEADS_A,
                2 * WIDTH_B, WIDTH_B, WIDTH_B, 2 * N_DIR * N_HEADS_B, 2 * D_MODEL)
D_IN = sum(SPLIT_WIDTHS)
SPLIT_POINTS = tuple(sum(SPLIT_WIDTHS[: i + 1]) for i in range(len(SPLIT_WIDTHS) - 1))

kernel_name = "hybrid_gdn_mlstm_hiermoe_adaln"


def rmsnorm(x, g):
    xf = x.astype(jnp.float32)
    y = xf * lax.rsqrt(jnp.mean(xf * xf, axis=-1, keepdims=True) + EPS)
    return y * g.astype(jnp.float32)


def l2norm(t):
    return t * lax.rsqrt(jnp.sum(t * t, axis=-1, keepdims=True) + EPS)


def head_layernorm(t, g):
    mu = jnp.mean(t, axis=-1, keepdims=True)
    tc = t - mu
    return tc * lax.rsqrt(jnp.mean(tc * tc, axis=-1, keepdims=True) + EPS) * g


def centred_conv(x, w):
    c = x.shape[-1]
    return lax.conv_general_dilated(
        x, w[:, None, :].astype(x.dtype), window_strides=(1,),
        padding=[(CONV_K // 2, CONV_K // 2)],
        dimension_numbers=("NWC", "WIO", "NWC"), feature_group_count=c)


def chunk_heads(t):
    b, s, h, d = t.shape
    return t.reshape(b, s // CHUNK, CHUNK, h, d).transpose(0, 3, 1, 2, 4)


def chunk_scalar(t):
    b, s, h = t.shape
    return t.reshape(b, s // CHUNK, CHUNK, h).transpose(0, 3, 1, 2)


def unchunk(t):
    b, h, n, l, d = t.shape
    return t.transpose(0, 2, 3, 1, 4).reshape(b, n * l, h, d)


def flip_seq(t):
    return jnp.flip(t, axis=1)


def gated_delta_chunked(q, k, v, g, beta):
    L = q.shape[-2]
    dv = v.shape[-1]
    b, h, n, _, dk = k.shape
    tri_incl = jnp.tril(jnp.ones((L, L), dtype=bool))
    tri_strict = jnp.tril(jnp.ones((L, L), dtype=bool), -1)
    g_cum = jnp.cumsum(g, axis=-1)
    decay = jnp.exp(jnp.where(tri_incl, g_cum[..., :, None] - g_cum[..., None, :], NEG_INF))
    k_beta = k * beta[..., None]
    lower = jnp.where(tri_strict, jnp.einsum("bhnid,bhnjd->bhnij", k_beta, k) * decay, 0.0)
    lhs = lower + jnp.eye(L, dtype=lower.dtype)
    rhs = jnp.concatenate([v * beta[..., None], k_beta * jnp.exp(g_cum)[..., None]], axis=-1)
    sol = lax.linalg.triangular_solve(lhs, rhs, left_side=True, lower=True, unit_diagonal=True)
    u, w = sol[..., :dv], sol[..., dv:]
    k_to_end = k * jnp.exp(g_cum[..., -1:] - g_cum)[..., None]
    chunk_decay = jnp.exp(g_cum[..., -1])

    def step(state, xs):
        u_c, w_c, kd_c, cd_c = xs
        v_new = u_c - jnp.einsum("bhlk,bhkv->bhlv", w_c, state)
        state_next = state * cd_c[..., None, None] + jnp.einsum("bhlk,bhlv->bhkv", kd_c, v_new)
        return state_next, (state, v_new)

    s0 = jnp.zeros((b, h, dk, dv), jnp.float32)
    xs = tuple(jnp.moveaxis(t, 2, 0) for t in (u, w, k_to_end, chunk_decay))
    _, (s_prev, v_new) = lax.scan(step, s0, xs)
    s_prev = jnp.moveaxis(s_prev, 0, 2)
    v_new = jnp.moveaxis(v_new, 0, 2)
    intra = jnp.einsum("bhnid,bhnjd->bhnij", q, k) * decay
    return (jnp.einsum("bhnlk,bhnkv->bhnlv", q * jnp.exp(g_cum)[..., None], s_prev)
            + jnp.einsum("bhnij,bhnjv->bhniv", intra, v_new))


def mlstm_chunked(q, k, v, i_pre, f_pre):
    L = q.shape[-2]
    b, h, n, _, dk = k.shape
    dv = v.shape[-1]
    tri_incl = jnp.tril(jnp.ones((L, L), dtype=bool))
    bcum = jnp.cumsum(jax.nn.log_sigmoid(f_pre), axis=-1)
    b_last = bcum[..., -1]
    a_end = b_last[..., None] - bcum + i_pre

    def step(carry, xs):
        c_st, n_st, m_st = carry
        k_c, v_c, a_c, bl_c = xs
        m_next = jnp.maximum(bl_c + m_st, jnp.max(a_c, axis=-1))
        scale_prev = jnp.exp(bl_c + m_st - m_next)
        wgt = jnp.exp(a_c - m_next[..., None])
        c_next = c_st * scale_prev[..., None, None] + jnp.einsum("bhlk,bhlv->bhkv", k_c * wgt[..., None], v_c)
        n_next = n_st * scale_prev[..., None] + jnp.einsum("bhl,bhlk->bhk", wgt, k_c)
        return (c_next, n_next, m_next), (c_st, n_st, m_st)

    init = (jnp.zeros((b, h, dk, dv), jnp.float32), jnp.zeros((b, h, dk), jnp.float32),
            jnp.zeros((b, h), jnp.float32))
    xs = tuple(jnp.moveaxis(t, 2, 0) for t in (k, v, a_end, b_last))
    _, (c_prev, n_prev, m_prev) = lax.scan(step, init, xs)
    c_prev = jnp.moveaxis(c_prev, 0, 2)
    n_prev = jnp.moveaxis(n_prev, 0, 2)
    m_prev = jnp.moveaxis(m_prev, 0, 2)
    log_d = jnp.where(tri_incl, bcum[..., :, None] - bcum[..., None, :] + i_pre[..., None, :], NEG_INF)
    log_inter = bcum + m_prev[..., None]
    m_t = jnp.maximum(log_inter, jnp.max(log_d, axis=-1))
    d_mat = jnp.exp(log_d - m_t[..., None])
    inter_w = jnp.exp(log_inter - m_t)
    qk = jnp.einsum("bhntd,bhnsd->bhnts", q, k) * d_mat
    num = (jnp.einsum("bhnts,bhnsv->bhntv", qk, v)
           + inter_w[..., None] * jnp.einsum("bhntk,bhnkv->bhntv", q, c_prev))
    den = jnp.sum(qk, axis=-1) + inter_w * jnp.einsum("bhntk,bhnk->bhnt", q, n_prev)
    return num / jnp.maximum(jnp.abs(den), jnp.exp(-m_t))[..., None]


def run_gdn(q, k, v, g, beta):
    return unchunk(gated_delta_chunked(chunk_heads(q), chunk_heads(k), chunk_heads(v),
                                       chunk_scalar(g), chunk_scalar(beta)))


def run_mlstm(q, k, v, i_pre, f_pre):
    return unchunk(mlstm_chunked(chunk_heads(q), chunk_heads(k), chunk_heads(v),
                                 chunk_scalar(i_pre), chunk_scalar(f_pre)))


def hybrid_mixer(h, w_in, gdn_conv_w, gdn_a_log, gdn_dt_bias, gdn_norm_g,
                 mlstm_conv_w, mlstm_i_bias, mlstm_f_bias, mlstm_norm_g,
                 w_branch_a, w_branch_b, w_out):
    bsz, s, _ = h.shape
    proj = jnp.einsum("bsd,de->bse", h, w_in.astype(jnp.float32))
    a_qkv, a_z, a_gates, b_qk, b_v, b_o, b_gates, merge = jnp.split(proj, SPLIT_POINTS, axis=-1)

    a_qkv = jax.nn.silu(centred_conv(a_qkv, gdn_conv_w.astype(jnp.float32)))
    aq, ak, av = jnp.split(a_qkv, 3, axis=-1)
    aq = l2norm(aq.reshape(bsz, s, N_HEADS_A, HEAD_DIM_A)) * (HEAD_DIM_A ** -0.5)
    ak = l2norm(ak.reshape(bsz, s, N_HEADS_A, HEAD_DIM_A))
    av = av.reshape(bsz, s, N_HEADS_A, HEAD_DIM_A)
    a_gates = jnp.moveaxis(a_gates.reshape(bsz, s, N_DIR, 2, N_HEADS_A), 2, 0)
    g = -jnp.exp(gdn_a_log.astype(jnp.float32))[:, None, None, :] * jax.nn.softplus(
        a_gates[..., 0, :] + gdn_dt_bias.astype(jnp.float32)[:, None, None, :])
    beta = jax.nn.sigmoid(a_gates[..., 1, :])
    o_a = (run_gdn(aq, ak, av, g[0], beta[0])
           + flip_seq(run_gdn(flip_seq(aq), flip_seq(ak), flip_seq(av), flip_seq(g[1]), flip_seq(beta[1]))))
    o_a = rmsnorm(o_a, gdn_norm_g) * jax.nn.silu(a_z.reshape(bsz, s, N_HEADS_A, HEAD_DIM_A))
    o_a = o_a.reshape(bsz, s, WIDTH_A)

    b_qk = jax.nn.silu(centred_conv(b_qk, mlstm_conv_w.astype(jnp.float32)))
    bq, bk = jnp.split(b_qk, 2, axis=-1)
    bq = bq.reshape(bsz, s, N_HEADS_B, HEAD_DIM_B)
    bk = bk.reshape(bsz, s, N_HEADS_B, HEAD_DIM_B) * (HEAD_DIM_B ** -0.5)
    bv = b_v.reshape(bsz, s, N_HEADS_B, HEAD_DIM_B)
    b_gates = jnp.moveaxis(b_gates.reshape(bsz, s, N_DIR, 2, N_HEADS_B), 2, 0)
    i_pre = b_gates[..., 0, :] + mlstm_i_bias.astype(jnp.float32)[:, None, None, :]
    f_pre = b_gates[..., 1, :] + mlstm_f_bias.astype(jnp.float32)[:, None, None, :]
    h_b = (run_mlstm(bq, bk, bv, i_pre[0], f_pre[0])
           + flip_seq(run_mlstm(flip_seq(bq), flip_seq(bk), flip_seq(bv), flip_seq(i_pre[1]), flip_seq(f_pre[1]))))
    h_b = head_layernorm(h_b, mlstm_norm_g.astype(jnp.float32).reshape(N_HEADS_B, HEAD_DIM_B))
    h_b = (h_b * jax.nn.sigmoid(b_o.reshape(bsz, s, N_HEADS_B, HEAD_DIM_B))).reshape(bsz, s, WIDTH_B)

    gate_a, gate_b = jnp.split(jax.nn.sigmoid(merge), 2, axis=-1)
    y = (gate_a * jnp.einsum("bse,ed->bsd", o_a, w_branch_a.astype(jnp.float32))
         + gate_b * jnp.einsum("bse,ed->bsd", h_b, w_branch_b.astype(jnp.float32)))
    return jnp.einsum("bsd,de->bse", y, w_out.astype(jnp.float32))


def hierarchical_moe(h, router_group, router_expert, w_gate, w_up, w_down):
    bsz, s, d = h.shape
    ht = h.reshape(bsz * s, d)
    group_logits = ht @ router_group.astype(jnp.float32)
    group_probs = jax.nn.softmax(group_logits, axis=-1)
    p_group, g_sel = lax.top_k(group_probs, 1)
    expert_logits = (ht @ router_expert.astype(jnp.float32)).reshape(-1, N_GROUPS, EXPERTS_PER_GROUP)
    in_group = jnp.take_along_axis(expert_logits, g_sel[:, :, None], axis=1)[:, 0]
    top_vals, top_idx = lax.top_k(in_group, TOP_K)
    top_w = jax.nn.softmax(top_vals, axis=-1) * p_group
    expert_id = g_sel * EXPERTS_PER_GROUP + top_idx
    combine = jnp.sum(jax.nn.one_hot(expert_id, N_EXPERTS, dtype=jnp.float32) * top_w[..., None], axis=1)
    y = jnp.zeros_like(ht)
    for grp in range(N_GROUPS):
        sl = slice(grp * EXPERTS_PER_GROUP, (grp + 1) * EXPERTS_PER_GROUP)
        hg = jnp.einsum("td,edf->tef", ht, w_gate[sl].astype(jnp.float32))
        hu = jnp.einsum("td,edf->tef", ht, w_up[sl].astype(jnp.float32))
        act = jax.nn.silu(hg) * hu * combine[:, sl, None]
        y = y + jnp.einsum("tef,efd->td", act, w_down[sl].astype(jnp.float32))
    return y.reshape(bsz, s, d)


def setup_inputs(seed: int = 0) -> dict:
    key = jax.random.key(seed)
    ks = jax.random.split(key, 24)
    nrm = lambda k, shape, scale: jax.random.normal(k, shape, jnp.float32) * scale
    gain = lambda k, shape: 1.0 + 0.02 * jax.random.normal(k, shape, jnp.float32)
    dt = jnp.exp(jax.random.uniform(ks[9], (DEPTH, N_DIR, N_HEADS_A), jnp.float32,
                                    np.log(1e-3), np.log(1e-1)))
    return {
        "x": nrm(ks[0], (BATCH, SEQ, D_MODEL), 1.0),
        "c": nrm(ks[1], (BATCH, D_MODEL), 1.0),
        "ada_w": nrm(ks[2], (DEPTH, D_MODEL, N_MOD * D_MODEL), 0.5 * D_MODEL ** -0.5),
        "ada_b": nrm(ks[3], (DEPTH, N_MOD * D_MODEL), 0.01),
        "norm_mix_g": gain(ks[4], (DEPTH, D_MODEL)),
        "norm_ffn_g": gain(ks[5], (DEPTH, D_MODEL)),
        "w_in": nrm(ks[6], (DEPTH, D_MODEL, D_IN), D_MODEL ** -0.5),
        "gdn_conv_w": nrm(ks[7], (DEPTH, CONV_K, 3 * WIDTH_A), CONV_K ** -0.5),
        "gdn_a_log": jnp.log(jax.random.uniform(ks[8], (DEPTH, N_DIR, N_HEADS_A), jnp.float32, 1.0, 16.0)),
        "gdn_dt_bias": jnp.log(jnp.expm1(dt)),
        "gdn_norm_g": gain(ks[10], (DEPTH, HEAD_DIM_A)),
        "mlstm_conv_w": nrm(ks[11], (DEPTH, CONV_K, 2 * WIDTH_B), CONV_K ** -0.5),
        "mlstm_i_bias": nrm(ks[12], (DEPTH, N_DIR, N_HEADS_B), 0.1),
        "mlstm_f_bias": 3.0 + 3.0 * jax.random.uniform(ks[13], (DEPTH, N_DIR, N_HEADS_B), jnp.float32),
        "mlstm_norm_g": gain(ks[14], (DEPTH, WIDTH_B)),
        "w_branch_a": nrm(ks[15], (DEPTH, WIDTH_A, D_MODEL), WIDTH_A ** -0.5),
        "w_branch_b": nrm(ks[16], (DEPTH, WIDTH_B, D_MODEL), WIDTH_B ** -0.5),
        "w_out": nrm(ks[17], (DEPTH, D_MODEL, D_MODEL), D_MODEL ** -0.5),
        "router_group": nrm(ks[18], (DEPTH, D_MODEL, N_GROUPS), D_MODEL ** -0.5),
        "router_expert": nrm(ks[19], (DEPTH, D_MODEL, N_EXPERTS), D_MODEL ** -0.5),
        "w_gate": nrm(ks[20], (DEPTH, N_EXPERTS, D_MODEL, D_EXPERT), D_MODEL ** -0.5),
        "w_up": nrm(ks[21], (DEPTH, N_EXPERTS, D_MODEL, D_EXPERT), D_MODEL ** -0.5),
        "w_down": nrm(ks[22], (DEPTH, N_EXPERTS, D_EXPERT, D_MODEL), D_EXPERT ** -0.5),
        "final_norm_g": gain(ks[23], (D_MODEL,)),
    }


def reference(x, c, ada_w, ada_b, norm_mix_g, norm_ffn_g, w_in, gdn_conv_w, gdn_a_log,
              gdn_dt_bias, gdn_norm_g, mlstm_conv_w, mlstm_i_bias, mlstm_f_bias, mlstm_norm_g,
              w_branch_a, w_branch_b, w_out, router_group, router_expert, w_gate, w_up, w_down,
              final_norm_g):
    out_dtype = x.dtype
    h_res = x.astype(jnp.float32)
    c_act = jax.nn.silu(c.astype(jnp.float32))
    for l in range(DEPTH):
        mod = c_act @ ada_w[l].astype(jnp.float32) + ada_b[l].astype(jnp.float32)
        sh1, sc1, gt1, sh2, sc2, gt2 = [m[:, None, :] for m in jnp.split(mod, N_MOD, axis=-1)]
        hn = rmsnorm(h_res, norm_mix_g[l]) * (1.0 + sc1) + sh1
        h_res = h_res + gt1 * hybrid_mixer(
            hn, w_in[l], gdn_conv_w[l], gdn_a_log[l], gdn_dt_bias[l], gdn_norm_g[l],
            mlstm_conv_w[l], mlstm_i_bias[l], mlstm_f_bias[l], mlstm_norm_g[l],
            w_branch_a[l], w_branch_b[l], w_out[l])
        hn = rmsnorm(h_res, norm_ffn_g[l]) * (1.0 + sc2) + sh2
        h_res = h_res + gt2 * hierarchical_moe(hn, router_group[l], router_expert[l],
                                               w_gate[l], w_up[l], w_down[l])
    return rmsnorm(h_res, final_norm_g).astype(out_dtype)
```

```python
import numpy as np
import concourse.bass as bass
import concourse.mybir as mybir

ENG_NAMES = ("pe", "act", "dve", "pool", "sp")


class Buf:
    __slots__ = ("name", "lw", "rd", "sem", "dma_cnt", "last_dma")

    def __init__(self, name):
        self.name = name
        self.lw = None
        self.rd = {}
        self.sem = None
        self.dma_cnt = 0
        self.last_dma = None


class Prog:
    def __init__(self, nc, stack):
        self.nc = nc
        self.stack = stack
        self.engs = {}
        for n in ENG_NAMES:
            self.engs[n] = dict(name=n, ops=[], cnt=0, sem=None,
                                waited={})
        self.nsem = 0
        for n in ("pe", "act", "dve", "pool"):
            self.engs[n]["sem"] = self._new_sem("e_" + n)
        self.n_instr = 0
        self.owners = []
        self.free_sems = []

    def _new_sem(self, name):
        self.nsem += 1
        assert self.nsem <= 100, "too many semaphores"
        return self.stack.enter_context(self.nc.semaphore(name))

    def _need(self, eng, tok, waits):
        if tok is None:
            return
        sem, val = tok
        key = id(sem)
        if eng["waited"].get(key, 0) >= val:
            return
        cur = waits.get(key)
        if cur is None or cur[1] < val:
            waits[key] = (sem, val)

    def _collect(self, eng, reads, writes, same_eng_war=False):
        waits = {}
        for b in reads:
            self._need(eng, b.lw, waits)
        for b in writes:
            self._need(eng, b.lw, waits)
            for k, t in b.rd.items():
                self._need(eng, t, waits)
        return waits

    def _emit_waits(self, eng, waits):
        lst = []
        for key, (sem, val) in waits.items():
            eng["waited"][key] = val
            lst.append((sem, val))
        return lst

    def op(self, engname, fn, reads=(), writes=()):
        signal = True
        eng = self.engs[engname]
        waits = self._collect(eng, reads, writes)
        wl = self._emit_waits(eng, waits)
        if signal:
            eng["cnt"] += 1
            tok = (eng["sem"], eng["cnt"])
        else:
            tok = (eng["sem"], eng["cnt"] + 1)
        eng["ops"].append(("op", wl, fn, signal))
        for b in reads:
            b.rd[engname] = tok
        for b in writes:
            b.lw = tok
            b.rd = {}
        self.n_instr += 1
        return tok

    def dma(self, qname, pairs, owner, reads=(), writes=(), **kw):
        eng = self.engs[qname]
        if owner.sem is None:
            if self.free_sems:
                owner.sem, owner.dma_cnt = self.free_sems.pop()
            else:
                owner.sem = self._new_sem("dsem%d" % self.nsem)
                owner.dma_cnt = 0
            owner.last_dma = None
            self.owners.append(owner)
        waits = self._collect(eng, reads, writes)
        if owner.last_dma is not None:
            self._need(eng, owner.last_dma, waits)
        wl = self._emit_waits(eng, waits)
        owner.dma_cnt += len(pairs)
        tok = (owner.sem, 16 * owner.dma_cnt)
        owner.last_dma = tok
        eng["ops"].append(("dma", wl, pairs, owner.sem, kw))
        for b in reads:
            b.rd["dma%d" % id(owner)] = tok
        for b in writes:
            b.lw = tok
            b.rd = {}
        self.n_instr += len(pairs)
        return tok

    def barrier(self):
        toks = []
        for n in ("pe", "act", "dve", "pool"):
            e = self.engs[n]
            if e["cnt"] > 0:
                toks.append((e["sem"], e["cnt"]))
        for b in self.owners:
            if b.dma_cnt > 0:
                toks.append((b.sem, 16 * b.dma_cnt))
        for n in ENG_NAMES:
            eng = self.engs[n]
            waits = {}
            for t in toks:
                self._need(eng, t, waits)
            wl = self._emit_waits(eng, waits)
            if wl:
                eng["ops"].append(("wait", wl))
        for b in self.owners:
            self.free_sems.append((b.sem, b.dma_cnt))
            b.sem = None
            b.last_dma = None
        self.owners = []

    def finish(self, final_bufs):
        nc = self.nc
        eng = self.engs["sp"]
        waits = {}
        for b in final_bufs:
            self._need(eng, b.lw, waits)
        for n in ("pe", "act", "dve", "pool"):
            e = self.engs[n]
            if e["cnt"] > 0:
                self._need(eng, (e["sem"], e["cnt"]), waits)
        wl = self._emit_waits(eng, waits)
        eng["ops"].append(("wait", wl))

        def run_stream(handle, ops):
            for o in ops:
                kind = o[0]
                for (sem, val) in o[1]:
                    handle.wait_ge(sem, val)
                if kind == "op":
                    ins = o[2](handle)
                    if o[3]:
                        ins.then_inc(self._engsem(handle), 1)
                elif kind == "dma":
                    pairs, sem, kw = o[2], o[3], o[4]
                    for (oap, iap) in pairs:
                        handle.dma_start(out=oap, in_=iap, **kw).then_inc(sem, 16)

        with nc.Block() as block:
            self._cur = {}

            @block.tensor
            def _(h):
                self._cur[id(h)] = self.engs["pe"]["sem"]
                run_stream(h, self.engs["pe"]["ops"])

            @block.scalar
            def _(h):
                self._cur[id(h)] = self.engs["act"]["sem"]
                run_stream(h, self.engs["act"]["ops"])

            @block.vector
            def _(h):
                self._cur[id(h)] = self.engs["dve"]["sem"]
                run_stream(h, self.engs["dve"]["ops"])

            @block.gpsimd
            def _(h):
                self._cur[id(h)] = self.engs["pool"]["sem"]
                run_stream(h, self.engs["pool"]["ops"])

            @block.sync
            def _(h):
                self._cur[id(h)] = None
                run_stream(h, self.engs["sp"]["ops"])

    def _engsem(self, handle):
        return self._cur[id(handle)]

import numpy as np
from contextlib import ExitStack
import concourse.bass as bass
import concourse.mybir as mybir

F32 = mybir.dt.float32
BF16 = mybir.dt.bfloat16
AF = mybir.ActivationFunctionType
ALU = mybir.AluOpType
AX = mybir.AxisListType

T = 4096
D = 1024
NT = T // 128
DEPTH = 2
EPS = 1e-6
NST = 64
C_AQKV, C_AZ, C_AG, C_BQK, C_BV, C_BO, C_BG, C_MERGE = 0, 1536, 2048, 2080, 3104, 3616, 4128, 4160
D_IN = 6208


class Ctx:
    pass


def build(stop_after="all", taps=(), depth=DEPTH, mxstop=None, nheads=8, stop_layer=0, layers=None, do_final=True, out_h=False):
    nc = bass.Bass("TRN2", target_bir_lowering=False)
    K = Ctx()
    K.nc = nc
    K.taps = set(taps)
    K.outs = {}
    K.mxstop = mxstop
    K.uid = 0
    K.ngroups = 8

    def din(name, shape, dt=F32):
        return nc.dram_tensor(name, list(shape), dt, kind="ExternalInput").ap()

    def dscr(name, shape, dt=F32):
        kind = "ExternalOutput" if name in K.taps else "Internal"
        t = nc.dram_tensor(name, list(shape), dt, kind=kind).ap()
        return t

    I = Ctx()
    I.x = din("x", [T, D])
    I.c_col = din("c_col", [128, 8])
    I.ada_w = din("ada_w", [DEPTH, D, 6 * D])
    I.ada_b = din("ada_b", [DEPTH, 128, 48])
    I.gmix = din("gmix", [DEPTH, 128, 8])
    I.gffn = din("gffn", [DEPTH, 128, 8])
    I.gfin = din("gfin", [128, 8])
    I.w_in = din("w_in", [DEPTH, D, D_IN])
    I.w_branch_a = din("w_branch_a", [DEPTH, 512, D])
    I.w_branch_b = din("w_branch_b", [DEPTH, 512, D])
    I.w_out = din("w_out", [DEPTH, D, D])
    I.router_group = din("router_group", [DEPTH, D, 4])
    I.router_expert = din("router_expert", [DEPTH, D, 32])
    I.w_gate = din("w_gate", [DEPTH, 32, D, 256])
    I.w_up = din("w_up", [DEPTH, 32, D, 256])
    I.w_down = din("w_down", [DEPTH, 32, 256, D])
    I.gfin_b = din("gfin_b", [128, D])
    I.convw = din("convw", [DEPTH, 128, 20, 5])
    I.gpar = din("gpar", [DEPTH, 128, 4, 8])
    I.gng = din("gng", [DEPTH, 2, 128, 4])
    out = nc.dram_tensor("out", [T, D], F32, kind="ExternalOutput").ap()

    S = Ctx()
    S.hnT = dscr("hnT", [8, 128, T], BF16)
    S.projT = dscr("projT", [32, 128, T], BF16)
    S.gcol = dscr("gcol", [128, NST * 32], F32)
    S.prepT = dscr("prepT", [20, 128, T], BF16)
    S.oaT = dscr("oaT", [4, 128, T], BF16)
    S.hbT = dscr("hbT", [4, 128, T], BF16)
    S.comb = dscr("comb", [128, NT * 32], F32)
    S.hA = [dscr("hA%d" % l, [T, D]) for l in range(DEPTH)]
    S.hB = [dscr("hB%d" % l, [T, D]) for l in range(DEPTH)]
    B = Ctx()
    for n in ("x", "w", "hnT", "projT", "gcol", "out", "prepT", "oaT", "hbT", "comb", "hA0", "hA1", "hB0", "hB1"):
        setattr(B, n, Buf("dram_" + n))

    with ExitStack() as st:
        P = Prog(nc, st)
        K.P = P

        def sb(name, shape, dt, stack=st):
            K.uid += 1
            t = stack.enter_context(nc.sbuf_tensor("s%d_%s" % (K.uid, name), list(shape), dt))
            return t, Buf(name)

        PS = []
        for i in range(8):
            t = st.enter_context(nc.psum_tensor("ps%d" % i, [128, 512], F32))
            PS.append((t, Buf("ps%d" % i)))
        K.ps_i = 0

        def nextps():
            r = PS[K.ps_i % 8]
            K.ps_i += 1
            return r

        K.ev_i = 0

        def evac_eng():
            K.ev_i += 1
            return "act" if (K.ev_i % 2) else "dve"

        ident, b_ident = sb("ident", [128, 128], F32)
        P.op("pool", lambda h: h.memset(ident[:], 0.0), writes=[b_ident])
        P.op("pool", lambda h: h.affine_select(out=ident[:], in_=ident[:], pattern=[[-1, 128]],
                                               compare_op=ALU.not_equal, fill=1.0, base=0,
                                               channel_multiplier=1), reads=[b_ident], writes=[b_ident])

        c_act, b_cact = sb("c_act", [128, 8], F32)
        P.dma("sp", [(c_act[:], I.c_col)], owner=b_cact, writes=[b_cact])
        P.op("act", lambda h: h.activation(out=c_act[:], in_=c_act[:], func=AF.Silu), reads=[b_cact], writes=[b_cact])
        modT = [sb("modT%d" % l, [128, 48], F32) for l in range(depth)]
        with ExitStack() as st0:
            was = [sb("wa%d" % i, [128, 8, 1024], F32, st0) for i in range(2)]
            adab, b_adab = sb("adab", [128, DEPTH, 48], F32, st0)
            P.dma("sp", [(adab[:, l, :], I.ada_b[l]) for l in range(DEPTH)], owner=b_adab, writes=[b_adab])
            gi = 0
            for l in range(depth):
                mt, b_mt = modT[l]
                pm, b_pm = nextps()
                for g in range(6):
                    wa, b_wa = was[gi % 2]
                    gi += 1
                    src = I.ada_w[l][:, g * 1024:(g + 1) * 1024].rearrange("(k p) n -> p k n", p=128)
                    P.dma("sp", [(wa[:, 0:4, :], src[:, 0:4, :]), (wa[:, 4:8, :], src[:, 4:8, :])], owner=b_wa, writes=[b_wa])

                    def mm(h, wa=wa, g=g, pm=pm):
                        ins = None
                        for jj in range(8):
                            j = g * 8 + jj
                            for k in range(8):
                                ins = h.matmul(pm[:, j:j + 1], lhsT=wa[:, k, jj * 128:(jj + 1) * 128],
                                               rhs=c_act[:, k:k + 1], start=(k == 0), stop=(k == 7))
                        return ins
                    P.op("pe", mm, reads=[b_wa, b_cact], writes=[b_pm])
                P.op("dve", lambda h, mt=mt, pm=pm, l=l: h.tensor_tensor(out=mt[:], in0=pm[:, 0:48], in1=adab[:, l, :], op=ALU.add),
                     reads=[b_pm, b_adab], writes=[b_mt])
        P.barrier()
        if "modT" in K.taps:
            for l in range(depth):
                o = nc.dram_tensor("modT%d_o" % l, [128, 48], F32, kind="ExternalOutput").ap()
                bo = Buf("modTo%d" % l)
                P.dma("sp", [(o, modT[l][0][:])], owner=modT[l][1], reads=[modT[l][1]], writes=[bo])
                K.outs["modT%d_o" % l] = bo

        gmix, b_gmix = sb("gmix", [128, DEPTH, 8], F32)
        gffn, b_gffn = sb("gffn", [128, DEPTH, 8], F32)
        gfin, b_gfin = sb("gfin", [128, 8], F32)
        P.dma("sp", [(gmix[:, l, :], I.gmix[l]) for l in range(DEPTH)], owner=b_gmix, writes=[b_gmix])
        P.dma("sp", [(gffn[:, l, :], I.gffn[l]) for l in range(DEPTH)], owner=b_gffn, writes=[b_gffn])
        P.dma("sp", [(gfin[:], I.gfin)], owner=b_gfin, writes=[b_gfin])

        def stage_norm(h_src, b_hsrc, gs_ap, sh_ap, b_par, hnT_dst, b_dst, tag):
            with ExitStack() as s1:
                xts = [sb("xt%s%d" % (tag, i), [128, D], F32, s1) for i in range(2)]
                xss = [sb("xs%s%d" % (tag, i), [128, D], F32, s1) for i in range(2)]
                sts = [sb("st%s%d" % (tag, i), [128, 4], F32, s1) for i in range(2)]
                hts = [sb("ht%s%d" % (tag, i), [128, 8, 512], BF16, s1) for i in range(2)]
                junk, b_junk = sb("junk" + tag, [128, D], BF16, s1)
                for t in range(NT):
                    xt, b_xt = xts[t % 2]
                    xs, b_xs = xss[t % 2]
                    stt, b_st = sts[t % 2]
                    ht, b_ht = hts[(t // 4) % 2]
                    P.dma("sp", [(xt[:], h_src[t * 128:(t + 1) * 128, :])], owner=b_xt, reads=[b_hsrc], writes=[b_xt])
                    P.op("act", lambda h, xt=xt, stt=stt: h.activation(out=junk[:], in_=xt[:], func=AF.Square, accum_out=stt[:, 0:1]),
                         reads=[b_xt], writes=[b_junk, b_st])
                    P.op("act", lambda h, stt=stt: h.activation(out=stt[:, 1:2], in_=stt[:, 0:1], func=AF.Sqrt, scale=1.0 / D, bias=eps_t[:]),
                         reads=[b_st, b_eps], writes=[b_st])
                    P.op("dve", lambda h, stt=stt: h.reciprocal(out=stt[:, 2:3], in_=stt[:, 1:2]), reads=[b_st], writes=[b_st])
                    P.op("dve", lambda h, xs=xs, xt=xt, stt=stt: h.tensor_scalar(out=xs[:], in0=xt[:], scalar1=stt[:, 2:3], scalar2=None, op0=ALU.mult),
                         reads=[b_xt, b_st], writes=[b_xs])
                    for half in range(2):
                        pt, b_pt = nextps()

                        def tr(h, pt=pt, xs=xs, half=half):
                            ins = None
                            for q in range(4):
                                k = half * 4 + q
                                ins = h.transpose(pt[:, q * 128:(q + 1) * 128], xs[:, k * 128:(k + 1) * 128], ident[:])
                            return ins
                        P.op("pe", tr, reads=[b_xs, b_ident], writes=[b_pt])
                        for q in range(4):
                            k = half * 4 + q
                            dst = ht[:, k, (t % 4) * 128:(t % 4 + 1) * 128]
                            src = pt[:, q * 128:(q + 1) * 128]
                            if (q % 2) == 0:
                                if sh_ap is not None:
                                    P.op("act", lambda h, dst=dst, src=src, k=k: h.activation(out=dst, in_=src, func=AF.Identity, scale=gs_ap[:, k:k + 1], bias=sh_ap[:, k:k + 1]),
                                         reads=[b_pt, b_par], writes=[b_ht])
                                else:
                                    P.op("act", lambda h, dst=dst, src=src, k=k: h.activation(out=dst, in_=src, func=AF.Copy, scale=gs_ap[:, k:k + 1]),
                                         reads=[b_pt, b_par], writes=[b_ht])
                            else:
                                if sh_ap is not None:
                                    P.op("dve", lambda h, dst=dst, src=src, k=k: h.tensor_scalar(out=dst, in0=src, scalar1=gs_ap[:, k:k + 1], scalar2=sh_ap[:, k:k + 1], op0=ALU.mult, op1=ALU.add),
                                         reads=[b_pt, b_par], writes=[b_ht])
                                else:
                                    P.op("dve", lambda h, dst=dst, src=src, k=k: h.tensor_scalar(out=dst, in0=src, scalar1=gs_ap[:, k:k + 1], scalar2=None, op0=ALU.mult),
                                         reads=[b_pt, b_par], writes=[b_ht])
                    if t % 4 == 3:
                        tb = t // 4
                        P.dma("sp", [(hnT_dst[k, :, tb * 512:(tb + 1) * 512], ht[:, k, :]) for k in range(8)],
                              owner=b_ht, reads=[b_ht], writes=[b_dst])
            P.barrier()

        eps_t, b_eps = sb("eps_t", [128, 1], F32)
        P.op("dve", lambda h: h.memset(eps_t[:], EPS), writes=[b_eps])
        one_t, b_one = sb("one_t", [128, 1], F32)
        P.op("dve", lambda h: h.memset(one_t[:], 1.0), writes=[b_one])

        def stage_proj(l):
            with ExitStack() as s2:
                hn, b_hn = sb("hn", [128, 8, T], BF16, s2)
                P.dma("sp", [(hn[:, k, :], S.hnT[k]) for k in range(8)], owner=b_hn, reads=[B.hnT], writes=[b_hn])
                wts = [sb("wt%d" % i, [128, 8, 512], BF16, s2) for i in range(2)]
                ots = [sb("ot%d" % i, [128, T], BF16, s2) for i in range(2)]
                wg, b_wg = sb("wg", [128, 8, 64], BF16, s2)
                gc, b_gc = sb("gc", [128, NST * 32], F32, s2)
                wsrc = I.w_in[l].rearrange("(k p) n -> p k n", p=128)
                P.dma("pool", [(wg[:, :, 0:16], wsrc[:, :, C_AG:C_AG + 16]), (wg[:, :, 16:32], wsrc[:, :, C_BG:C_BG + 16]),
                               (wg[:, :, 32:48], wsrc[:, :, C_AG + 16:C_AG + 32]), (wg[:, :, 48:64], wsrc[:, :, C_BG + 16:C_BG + 32])],
                      owner=b_wg, reads=[B.w], writes=[b_wg])
                if K.mxstop == "p_hn" and l == stop_layer:
                    P.barrier()
                    return
                for sg in range(NST // 16):
                    pg, b_pg = nextps()

                    def mmg(h, pg=pg, sg=sg):
                        ins = None
                        for si in range(16):
                            s = sg * 16 + si
                            for k in range(8):
                                h.matmul(pg[0:64, si * 32:(si + 1) * 32], lhsT=hn[:, k, s * 64:(s + 1) * 64], rhs=wg[:, k, 0:32],
                                         start=(k == 0), stop=(k == 7))
                            for k in range(8):
                                ins = h.matmul(pg[64:128, si * 32:(si + 1) * 32], lhsT=hn[:, k, (63 - s) * 64:(64 - s) * 64], rhs=wg[:, k, 32:64],
                                               start=(k == 0), stop=(k == 7), tile_position=(0, 64))
                        return ins
                    P.op("pe", mmg, reads=[b_hn, b_wg], writes=[b_pg])
                    P.op("act", lambda h, pg=pg, sg=sg: h.copy(out=gc[:, sg * 512:(sg + 1) * 512], in_=pg[:, :]), reads=[b_pg], writes=[b_gc])
                P.dma("sp", [(S.gcol[:, 0:NST * 32], gc[:, 0:NST * 32])], owner=b_gc, reads=[b_gc], writes=[B.gcol])
                if K.mxstop == "p_gates" and l == stop_layer:
                    P.barrier()
                    return
                col_groups = [0, 512, 1024, 1536, C_BQK, C_BQK + 512, C_BV, C_BO]
                for gi, c0 in enumerate(col_groups):
                    wt, b_wt = wts[gi % 2]
                    P.dma("pool", [(wt[:, 0:4, :], wsrc[:, 0:4, c0:c0 + 512]), (wt[:, 4:8, :], wsrc[:, 4:8, c0:c0 + 512])],
                          owner=b_wt, reads=[B.w], writes=[b_wt])
                    for cc in range(4):
                        ot, b_ot = ots[(gi * 4 + cc) % 2]
                        for tb in range(8):
                            pp, b_pp = nextps()

                            def mm(h, pp=pp, wt=wt, cc=cc, tb=tb):
                                ins = None
                                for k in range(8):
                                    ins = h.matmul(pp[:, :], lhsT=wt[:, k, cc * 128:(cc + 1) * 128], rhs=hn[:, k, tb * 512:(tb + 1) * 512],
                                                   start=(k == 0), stop=(k == 7))
                                return ins
                            P.op("pe", mm, reads=[b_wt, b_hn], writes=[b_pp])
                            e = evac_eng()
                            if e == "act":
                                P.op("act", lambda h, ot=ot, pp=pp, tb=tb: h.copy(out=ot[:, tb * 512:(tb + 1) * 512], in_=pp[:, :]), reads=[b_pp], writes=[b_ot])
                            else:
                                P.op("dve", lambda h, ot=ot, pp=pp, tb=tb: h.tensor_copy(out=ot[:, tb * 512:(tb + 1) * 512], in_=pp[:, :]), reads=[b_pp], writes=[b_ot])
                        P.dma("sp", [(S.projT[gi * 4 + cc], ot[:])], owner=b_ot, reads=[b_ot], writes=[B.projT])
            P.barrier()

        def cbuf(name, shape, dt):
            return sb(name, shape, dt)

        def aff(ap, pattern, cm, base, op, fill, rb, wb):
            P.op("pool", lambda h: h.affine_select(out=ap, in_=ap, pattern=pattern, compare_op=op, fill=fill,
                                                   base=base, channel_multiplier=cm), reads=rb, writes=wb)

        identb, b_identb = cbuf("identb", [128, 128], BF16)
        P.op("dve", lambda h: h.tensor_copy(out=identb[:], in_=ident[:]), reads=[b_ident], writes=[b_identb])
        ident2, b_ident2 = cbuf("ident2", [128, 64], F32)
        P.op("dve", lambda h: h.tensor_copy(out=ident2[0:64, :], in_=ident[0:64, 0:64]), reads=[b_ident], writes=[b_ident2])
        P.op("dve", lambda h: h.tensor_copy(out=ident2[64:128, :], in_=ident[64:128, 64:128]), reads=[b_ident], writes=[b_ident2])
        ones_bd, b_onesbd = cbuf("ones_bd", [128, 128], F32)
        P.op("pool", lambda h: h.memset(ones_bd[:], 0.0), writes=[b_onesbd])
        P.op("pool", lambda h: h.memset(ones_bd[0:64, 0:64], 1.0), writes=[b_onesbd])
        P.op("pool", lambda h: h.memset(ones_bd[64:128, 64:128], 1.0), writes=[b_onesbd])
        bones, b_bones = cbuf("bones", [128, 128], BF16)
        P.op("dve", lambda h: h.tensor_copy(out=bones[:], in_=ones_bd[:]), reads=[b_onesbd], writes=[b_bones])
        tmpL, b_tmpL = cbuf("tmpL", [128, 64], F32)
        tmpU, b_tmpU = cbuf("tmpU", [128, 64], F32)

        def half_mask(dst, b_dst, lowers, uppers):
            P.op("pool", lambda h: h.memset(tmpL[:], 1.0), writes=[b_tmpL])
            for (pat, cm, base, op) in lowers:
                aff(tmpL[:], pat, cm, base, op, 0.0, [b_tmpL], [b_tmpL])
            P.op("pool", lambda h: h.memset(tmpU[:], 1.0), writes=[b_tmpU])
            for (pat, cm, base, op) in uppers:
                aff(tmpU[:], pat, cm, base, op, 0.0, [b_tmpU], [b_tmpU])
            P.op("pool", lambda h: h.tensor_copy(out=dst[0:64], in_=tmpL[0:64, :]), reads=[b_tmpL], writes=[b_dst])
            P.op("pool", lambda h: h.tensor_copy(out=dst[64:128], in_=tmpU[64:128, :]), reads=[b_tmpU], writes=[b_dst])

        maskI, b_maskI = cbuf("maskI", [128, 64], F32)
        maskS, b_maskS = cbuf("maskS", [128, 64], F32)
        half_mask(maskI[:, :], b_maskI, [([[1, 64]], -1, 0, ALU.is_ge)], [([[-1, 64]], 1, -64, ALU.is_ge)])
        half_mask(maskS[:, :], b_maskS, [([[1, 64]], -1, 0, ALU.is_gt)], [([[-1, 64]], 1, -64, ALU.is_gt)])
        tri_bd, b_tribd = cbuf("tri_bd", [128, 128], F32)
        P.op("pool", lambda h: h.memset(tri_bd[:], 0.0), writes=[b_tribd])
        P.op("pool", lambda h: h.tensor_copy(out=tri_bd[0:64, 0:64], in_=maskI[0:64, :]), reads=[b_maskI], writes=[b_tribd])
        P.op("pool", lambda h: h.tensor_copy(out=tri_bd[64:128, 64:128], in_=maskI[64:128, :]), reads=[b_maskI], writes=[b_tribd])
        bdk, b_bdk = cbuf("bdk", [128, 7, 64], F32)
        P.op("pool", lambda h: h.tensor_copy(out=bdk[:, 0, :], in_=ident2[:]), reads=[b_ident2], writes=[b_bdk])
        P.op("pool", lambda h: h.memset(bdk[:, 6, :], 1.0), writes=[b_bdk])
        for lv in range(1, 6):
            k = 1 << lv
            nq = 64 // k
            pat = [[-k, nq], [0, k]]
            tl3 = tmpL[:, :].rearrange("p (q r) -> p q r", r=k)
            tu3 = tmpU[:, :].rearrange("p (q r) -> p q r", r=k)
            P.op("pool", lambda h: h.memset(tmpL[:], 1.0), writes=[b_tmpL])
            aff(tl3, pat, 1, 0, ALU.is_ge, 0.0, [b_tmpL], [b_tmpL])
            aff(tl3, [[k, nq], [0, k]], -1, k, ALU.is_gt, 0.0, [b_tmpL], [b_tmpL])
            P.op("pool", lambda h: h.memset(tmpU[:], 1.0), writes=[b_tmpU])
            aff(tu3, pat, 1, -64, ALU.is_ge, 0.0, [b_tmpU], [b_tmpU])
            aff(tu3, [[k, nq], [0, k]], -1, 64 + k, ALU.is_gt, 0.0, [b_tmpU], [b_tmpU])
            P.op("pool", lambda h, lv=lv: h.tensor_copy(out=bdk[0:64, lv, :], in_=tmpL[0:64, :]), reads=[b_tmpL], writes=[b_bdk])
            P.op("pool", lambda h, lv=lv: h.tensor_copy(out=bdk[64:128, lv, :], in_=tmpU[64:128, :]), reads=[b_tmpU], writes=[b_bdk])
        mtl, b_mtl = cbuf("mtl", [128, 6, 64], BF16)
        mtmp, b_mtmp = cbuf("mtmp", [128, 6, 64], F32)
        P.op("pool", lambda h: h.tensor_tensor(out=mtmp[:], in0=bdk[:, 1:7, :], in1=bdk[:, 0:6, :], op=ALU.subtract), reads=[b_bdk], writes=[b_mtmp])
        P.op("pool", lambda h: h.tensor_tensor(out=mtl[:], in0=mtmp[:], in1=maskS[:].unsqueeze(1).to_broadcast([128, 6, 64]), op=ALU.mult),
             reads=[b_mtmp, b_maskS], writes=[b_mtl])
        ident2b, b_ident2b = cbuf("ident2b", [128, 64], BF16)
        P.op("dve", lambda h: h.tensor_copy(out=ident2b[:], in_=ident2[:]), reads=[b_ident2], writes=[b_ident2b])
        if "masks" in K.taps:
            o = nc.dram_tensor("masks_o", [128, 9, 64], F32, kind="ExternalOutput").ap()
            mo, b_mo = cbuf("mo", [128, 9, 64], F32)
            P.op("dve", lambda h: h.tensor_copy(out=mo[:, 0:6, :], in_=mtl[:]), reads=[b_mtl], writes=[b_mo])
            P.op("dve", lambda h: h.tensor_copy(out=mo[:, 6, :], in_=maskI[:]), reads=[b_maskI], writes=[b_mo])
            P.op("dve", lambda h: h.tensor_copy(out=mo[:, 7, :], in_=maskS[:]), reads=[b_maskS], writes=[b_mo])
            P.op("dve", lambda h: h.tensor_copy(out=mo[:, 8, :], in_=tri_bd[:, 32:96]), reads=[b_tribd], writes=[b_mo])
            bo = Buf("masks_o")
            P.dma("sp", [(o, mo[:])], owner=b_mo, reads=[b_mo], writes=[bo])
            K.outs["masks_o"] = bo
        P.barrier()

        def stage_prep(l):
            with ExitStack() as s3:
                cw, b_cw = sb("cw", [128, 20, 5], F32, s3)
                P.dma("sp", [(cw[:], I.convw[l])], owner=b_cw, writes=[b_cw])
                xps = [sb("xp%d" % i, [128, T + 4], BF16, s3) for i in range(2)]
                accs = [sb("acc%d" % i, [128, T], F32, s3) for i in range(2)]
                sq, b_sq = sb("sq", [128, T], BF16, s3)
                rn, b_rn = sb("rn", [128, T], F32, s3)
                outs = [sb("po%d" % i, [128, T], BF16, s3) for i in range(2)]
                for (xp, b_xp) in xps:
                    P.op("pool", lambda h, xp=xp: h.memset(xp[:], 0.0), writes=[b_xp])
                for ci in range(20):
                    src = ci if ci < 12 else 16 + (ci - 12)
                    xp, b_xp = xps[ci % 2]
                    acc, b_acc = accs[ci % 2]
                    po, b_po = outs[ci % 2]
                    P.dma("sp", [(xp[:, 2:T + 2], S.projT[src])], owner=b_xp, reads=[B.projT], writes=[b_xp])
                    P.op("dve", lambda h, acc=acc, xp=xp, ci=ci: h.tensor_scalar(out=acc[:], in0=xp[:, 0:T], scalar1=cw[:, ci, 0:1], scalar2=None, op0=ALU.mult),
                         reads=[b_xp, b_cw], writes=[b_acc])
                    for j in range(1, 5):
                        P.op("dve", lambda h, acc=acc, xp=xp, ci=ci, j=j: h.scalar_tensor_tensor(out=acc[:], in0=xp[:, j:T + j], scalar=cw[:, ci, j:j + 1], in1=acc[:], op0=ALU.mult, op1=ALU.add),
                             reads=[b_xp, b_cw, b_acc], writes=[b_acc])
                    P.op("act", lambda h, acc=acc: h.activation(out=acc[:], in_=acc[:], func=AF.Silu), reads=[b_acc], writes=[b_acc])
                    if ci < 8:
                        qscale = 0.125 if ci < 4 else 1.0
                        P.op("act", lambda h, acc=acc: h.activation(out=sq[:], in_=acc[:], func=AF.Square), reads=[b_acc], writes=[b_sq])
                        for blk in range(8):
                            pp, b_pp = nextps()
                            sl = slice(blk * 512, (blk + 1) * 512)
                            P.op("pe", lambda h, pp=pp, sl=sl: h.matmul(pp[:, :], lhsT=bones[:], rhs=sq[:, sl], start=True, stop=True), reads=[b_bones, b_sq], writes=[b_pp])
                            P.op("act", lambda h, pp=pp, sl=sl: h.activation(out=rn[:, sl], in_=pp[:, :], func=AF.Sqrt, bias=eps_t[:], scale=1.0), reads=[b_pp, b_eps], writes=[b_rn])
                        P.op("dve", lambda h: h.reciprocal(out=rn[:], in_=rn[:]), reads=[b_rn], writes=[b_rn])
                        P.op("dve", lambda h, po=po, acc=acc, qscale=qscale: h.scalar_tensor_tensor(out=po[:], in0=acc[:], scalar=qscale, in1=rn[:], op0=ALU.mult, op1=ALU.mult),
                             reads=[b_acc, b_rn], writes=[b_po])
                    else:
                        sc = 0.125 if ci >= 16 else 1.0
                        P.op("act", lambda h, po=po, acc=acc, sc=sc: h.activation(out=po[:], in_=acc[:], func=AF.Copy, scale=sc), reads=[b_acc], writes=[b_po])
                    P.dma("sp", [(S.prepT[ci], po[:])], owner=b_po, reads=[b_po], writes=[B.prepT])
            P.barrier()

        def stage_mixer(l, kind):
            is_g = (kind == "gdn")
            DV = 64 if is_g else 65
            CAP = 1.0 if is_g else 1e30
            with ExitStack() as s4:
                gc, b_gc = sb("m_gc", [128, NST, 32], F32, s4)
                P.dma("sp", [(gc[:].rearrange("p s c -> p (s c)"), S.gcol)], owner=b_gc, reads=[B.gcol], writes=[b_gc])
                gp, b_gp = sb("m_gp", [128, 4, 8], F32, s4)
                P.dma("sp", [(gp[:], I.gpar[l])], owner=b_gp, writes=[b_gp])
                gng, b_gng = sb("m_gng", [128, 4], F32, s4)
                P.dma("sp", [(gng[:], I.gng[l][0 if is_g else 1])], owner=b_gng, writes=[b_gng])
                c0 = 0 if is_g else 16
                ta, b_ta = sb("m_ta", [128, NST, 8], F32, s4)
                tb_, b_tb = sb("m_tb", [128, NST, 8], F32, s4)
                nea, b_nea = sb("m_nea", [128, 8], F32, s4)
                names = ["g", "beta", "gcs", "cj", "egc", "ekd", "cd", "aa"]
                Gt = {}
                for n in names:
                    Gt[n] = sb("m_G" + n, [128, 8, NST], F32, s4)

                def bc8(ap):
                    return ap.unsqueeze(1).to_broadcast([128, NST, 8])

                def to_hs(dst, src, b_dst, b_src, eng="dve"):
                    P.op(eng, lambda h: h.tensor_copy(out=dst[:], in_=src[:].rearrange("p s h -> p h s")), reads=[b_src], writes=[b_dst])

                if is_g:
                    P.op("dve", lambda h: h.tensor_tensor(out=ta[:], in0=gc[:, :, c0:c0 + 8], in1=bc8(gp[:, 0, :]), op=ALU.add), reads=[b_gc, b_gp], writes=[b_ta])
                    P.op("dve", lambda h: h.tensor_scalar_min(out=ta[:], in0=ta[:], scalar1=60.0), reads=[b_ta], writes=[b_ta])
                    P.op("act", lambda h: h.activation(out=ta[:], in_=ta[:], func=AF.Exp), reads=[b_ta], writes=[b_ta])
                    P.op("act", lambda h: h.activation(out=ta[:], in_=ta[:], func=AF.Ln, bias=one_t[:], scale=1.0), reads=[b_ta, b_one], writes=[b_ta])
                    P.op("act", lambda h: h.activation(out=nea[:], in_=gp[:, 1, :], func=AF.Exp), reads=[b_gp], writes=[b_nea])
                    P.op("dve", lambda h: h.tensor_scalar(out=nea[:], in0=nea[:], scalar1=-1.0, scalar2=None, op0=ALU.mult), reads=[b_nea], writes=[b_nea])
                    P.op("dve", lambda h: h.tensor_tensor(out=ta[:], in0=ta[:], in1=bc8(nea[:]), op=ALU.mult), reads=[b_ta, b_nea], writes=[b_ta])
                    to_hs(Gt["g"][0], ta, Gt["g"][1], b_ta)
                    P.op("act", lambda h: h.activation(out=tb_[:], in_=gc[:, :, c0 + 8:c0 + 16], func=AF.Sigmoid), reads=[b_gc], writes=[b_tb])
                    to_hs(Gt["beta"][0], tb_, Gt["beta"][1], b_tb)
                else:
                    P.op("dve", lambda h: h.tensor_tensor(out=ta[:], in0=gc[:, :, c0 + 8:c0 + 16], in1=bc8(gp[:, 3, :]), op=ALU.add), reads=[b_gc, b_gp], writes=[b_ta])
                    P.op("dve", lambda h: h.tensor_scalar_max(out=ta[:], in0=ta[:], scalar1=-60.0), reads=[b_ta], writes=[b_ta])
                    P.op("act", lambda h: h.activation(out=ta[:], in_=ta[:], func=AF.Exp, scale=-1.0), reads=[b_ta], writes=[b_ta])
                    P.op("act", lambda h: h.activation(out=ta[:], in_=ta[:], func=AF.Ln, bias=one_t[:], scale=1.0), reads=[b_ta, b_one], writes=[b_ta])
                    P.op("dve", lambda h: h.tensor_scalar(out=ta[:], in0=ta[:], scalar1=-1.0, scalar2=None, op0=ALU.mult), reads=[b_ta], writes=[b_ta])
                    to_hs(Gt["g"][0], ta, Gt["g"][1], b_ta)
                    P.op("dve", lambda h: h.tensor_tensor(out=tb_[:], in0=gc[:, :, c0:c0 + 8], in1=bc8(gp[:, 2, :]), op=ALU.add), reads=[b_gc, b_gp], writes=[b_tb])
                    to_hs(Gt["aa"][0], tb_, Gt["aa"][1], b_tb)
                g_hs, b_g = Gt["g"]
                gcs, b_gcs = Gt["gcs"]
                cj, b_cj = Gt["cj"]
                egc, b_egc = Gt["egc"]
                ekd, b_ekd = Gt["ekd"]
                cd, b_cd = Gt["cd"]
                beta, b_beta = Gt["beta"]
                aa, b_aa = Gt["aa"]
                gflat = g_hs[:].rearrange("p h s -> p (h s)")
                p1, b_p1 = nextps()
                P.op("pe", lambda h: h.matmul(p1[:, :], lhsT=tri_bd[:], rhs=gflat, start=True, stop=True), reads=[b_tribd, b_g], writes=[b_p1])
                P.op("act", lambda h: h.copy(out=gcs[:].rearrange("p h s -> p (h s)"), in_=p1[:, :]), reads=[b_p1], writes=[b_gcs])
                p2, b_p2 = nextps()
                P.op("pe", lambda h: h.matmul(p2[:, :], lhsT=ones_bd[:], rhs=gflat, start=True, stop=True), reads=[b_onesbd, b_g], writes=[b_p2])
                if is_g:
                    P.op("dve", lambda h: h.tensor_copy(out=cj[:], in_=gcs[:]), reads=[b_gcs], writes=[b_cj])
                else:
                    P.op("dve", lambda h: h.tensor_tensor(out=cj[:], in0=gcs[:], in1=aa[:], op=ALU.subtract), reads=[b_gcs, b_aa], writes=[b_cj])
                P.op("act", lambda h: h.activation(out=egc[:], in_=gcs[:], func=AF.Exp), reads=[b_gcs], writes=[b_egc])
                P.op("act", lambda h: h.activation(out=cd[:].rearrange("p h s -> p (h s)"), in_=p2[:, :], func=AF.Exp), reads=[b_p2], writes=[b_cd])
                P.op("dve", lambda h: h.tensor_tensor(out=ekd[:].rearrange("p h s -> p (h s)"), in0=p2[:, :], in1=cj[:].rearrange("p h s -> p (h s)"), op=ALU.subtract), reads=[b_p2, b_cj], writes=[b_ekd])
                P.op("act", lambda h: h.activation(out=ekd[:], in_=ekd[:], func=AF.Exp), reads=[b_ekd], writes=[b_ekd])

                if "gates" in K.taps and is_g:
                    o = nc.dram_tensor("gates_o", [128, 6, 512], F32, kind="ExternalOutput").ap()
                    bo = Buf("gates_o")
                    for i_, n_ in enumerate(["g", "beta", "gcs", "egc", "ekd", "cd"]):
                        P.dma("sp", [(o[:, i_, :], Gt[n_][0][:].rearrange("p h s -> p (h s)"))], owner=Gt[n_][1], reads=[Gt[n_][1]], writes=[bo])
                    K.outs["gates_o"] = bo
                if K.mxstop == "gates":
                    P.barrier()
                    return
                kT2, b_kT2 = sb("m_kT2", [128, NST, 64], BF16, s4)
                qT2, b_qT2 = sb("m_qT2", [128, NST, 64], BF16, s4)
                vT2, b_vT2 = sb("m_vT2", [128, NST, 64], BF16, s4)
                Rt, b_R = sb("m_R", [128, NST, 64 + DV], BF16, s4)
                kd, b_kd = sb("m_kd", [128, NST, 64], BF16, s4)
                intraT, b_intraT = sb("m_intraT", [128, NST, 64], BF16, s4)
                Ot, b_O = sb("m_O", [128, NST, DV], BF16 if is_g else F32, s4)
                osum, b_osum = sb("m_osum", [128, T], F32, s4)
                if is_g:
                    Vfin, b_Vfin = sb("m_Vfin", [128, NST, 64], BF16, s4)
                    ut, b_u = sb("m_u", [128, NST, 64], F32, s4)
                    wt_, b_w = sb("m_w", [128, NST, 64], BF16, s4)
                    wT, b_wT = sb("m_wT", [128, NST, 64], BF16, s4)
                    ET, b_ET = sb("m_ET", [128, 8, 6, 64], BF16, s4)
                    Vs = [sb("m_V%d" % i, [128, 8, 64], BF16, s4) for i in range(2)]
                    Ws = [sb("m_W%d" % i, [128, 8, 64], BF16, s4) for i in range(2)]
                    Fb, b_Fb = sb("m_Fb", [128, 8, 64], BF16, s4)
                    NTg, b_NT = sb("m_NT", [128, 8, 64], BF16, s4)
                    dTs, b_dTs = sb("m_dTs", [128, 8, 64], F32, s4)
                dg, b_dg = sb("m_dg", [128, 8, 64], F32, s4)
                Xg, b_X = sb("m_X", [128, 8, 64], F32, s4)
                dTi, b_dTi = sb("m_dTi", [128, 8, 64], F32, s4)
                St, b_S = sb("m_S", [128, DV], F32, s4)
                Sb, b_Sb = sb("m_Sb", [128, DV], BF16, s4)
                vnew, b_vnew = sb("m_vnew", [128, DV], BF16, s4)
                tq, b_tq = sb("m_tq", [128, DV], F32, s4)
                zt, b_zt = sb("m_zt", [128, T], BF16, s4)
                if not is_g:
                    P.op("pool", lambda h: h.memset(Rt[:, :, 64:65], 1.0), writes=[b_R])
                    den, b_den = sb("m_den", [128, NST, 1], F32, s4)
                    mu, b_mu = sb("m_mu", [128, 512], F32, s4)
                sqp, b_sqp = sb("m_sqp", [128, T], BF16, s4)
                if not is_g:
                    obf, b_obf = sb("m_obf", [128, T], BF16, s4)
                rnp, b_rnp = sb("m_rnp", [128, 512], F32, s4)
                oout, b_oout = sb("m_oout", [128, T], BF16, s4)

                def halves(h, fn):
                    ins = None
                    for hf in range(2):
                        ins = fn(slice(hf * 64, hf * 64 + 64), hf)
                    return ins

                kbase = 4 if is_g else 16
                qbase = 0 if is_g else 12
                for hd in range(nheads):
                    pair, sub = hd // 2, hd % 2
                    rows = slice(sub * 64, sub * 64 + 64)

                    def ld2(dst, b_dst, srcT, b_src):
                        fw = srcT[rows, :].rearrange("c (n l) -> c n l", l=64)
                        P.dma("sp", [(dst[0:64, :, :], fw), (dst[64:128, :, :], fw[:, ::-1, :])], owner=b_dst, reads=[b_src], writes=[b_dst])
                    ld2(kT2, b_kT2, S.prepT[kbase + pair], B.prepT)
                    ld2(qT2, b_qT2, S.prepT[qbase + pair], B.prepT)
                    if is_g:
                        ld2(vT2, b_vT2, S.prepT[8 + pair], B.prepT)
                    else:
                        ld2(vT2, b_vT2, S.projT[24 + pair], B.projT)
                    if K.mxstop == "load":
                        o = nc.dram_tensor("load_o", [128, NST * 64], BF16, kind="ExternalOutput").ap()
                        bo = Buf("load_o")
                        P.dma("sp", [(o, kT2[:].rearrange("p s i -> p (s i)"))], owner=b_kT2, reads=[b_kT2], writes=[bo])
                        K.outs["load_o"] = bo
                        P.barrier()
                        return
                    for gi in range(K.ngroups):
                        s0 = gi * 8
                        ss = slice(s0, s0 + 8)
                        pk, b_pk = nextps()
                        pv, b_pv = nextps()

                        def mm_t(h, pk=pk, pv=pv, s0=s0):
                            ins = None
                            for si in range(8):
                                for hf in range(2):
                                    hs = slice(hf * 64, hf * 64 + 64)
                                    h.matmul(pk[hs, si * 64:(si + 1) * 64], lhsT=kT2[hs, s0 + si, :], rhs=identb[hs, hs], start=True, stop=True)
                                    ins = h.matmul(pv[hs, si * 64:(si + 1) * 64], lhsT=vT2[hs, s0 + si, :], rhs=identb[hs, hs], start=True, stop=True)
                            return ins
                        P.op("pe", mm_t, reads=[b_kT2, b_vT2, b_identb], writes=[b_pk, b_pv])
                        pk3 = pk[:, :].rearrange("p (s d) -> p s d", d=64)
                        pv3 = pv[:, :].rearrange("p (s d) -> p s d", d=64)
                        P.op("act", lambda h, pv3=pv3, ss=ss: h.copy(out=Rt[:, ss, 0:64], in_=pv3), reads=[b_pv], writes=[b_R])
                        if is_g:
                            P.op("dve", lambda h, pk3=pk3, ss=ss, hd=hd: h.tensor_tensor(out=Rt[:, ss, 64:128], in0=pk3, in1=egc[:, hd, ss].unsqueeze(2).to_broadcast([128, 8, 64]), op=ALU.mult),
                                 reads=[b_pk, b_egc], writes=[b_R])
                        P.op("dve", lambda h, pk3=pk3, ss=ss, hd=hd: h.tensor_tensor(out=kd[:, ss, :], in0=pk3, in1=ekd[:, hd, ss].unsqueeze(2).to_broadcast([128, 8, 64]), op=ALU.mult),
                             reads=[b_pk, b_ekd], writes=[b_kd])
                        P.op("pool", lambda h, ss=ss, hd=hd: h.tensor_tensor(out=dg[:], in0=ident2[:].unsqueeze(1).to_broadcast([128, 8, 64]),
                                                                              in1=gcs[:, hd, ss].unsqueeze(2).to_broadcast([128, 8, 64]), op=ALU.mult),
                             reads=[b_ident2, b_gcs], writes=[b_dg])
                        pd, b_pd = nextps()
                        P.op("pe", lambda h, pd=pd: h.matmul(pd[:, :], lhsT=ones_bd[:], rhs=dg[:].rearrange("p s i -> p (s i)"), start=True, stop=True),
                             reads=[b_onesbd, b_dg], writes=[b_pd])
                        pd3 = pd[:, :].rearrange("p (s i) -> p s i", i=64)
                        P.op("dve", lambda h, pd3=pd3, ss=ss, hd=hd: h.tensor_tensor(out=Xg[:], in0=pd3, in1=cj[:, hd, ss].unsqueeze(2).to_broadcast([128, 8, 64]), op=ALU.subtract),
                             reads=[b_pd, b_cj], writes=[b_X])
                        P.op("act", lambda h: h.activation(out=Xg[:], in_=Xg[:], func=AF.Exp), reads=[b_X], writes=[b_X])
                        P.op("dve", lambda h: h.scalar_tensor_tensor(out=dTi[:], in0=Xg[:], scalar=CAP, in1=maskI[:].unsqueeze(1).to_broadcast([128, 8, 64]), op0=ALU.min, op1=ALU.mult),
                             reads=[b_X, b_maskI], writes=[b_dTi])
                        pkq, b_pkq = nextps()
                        if is_g:
                            pkk, b_pkk = nextps()

                        def mm_kq(h, pkq=pkq, s0=s0, pkk=(pkk if is_g else None)):
                            ins = None
                            for si in range(8):
                                for hf in range(2):
                                    hs = slice(hf * 64, hf * 64 + 64)
                                    if is_g:
                                        h.matmul(pkk[hs, si * 64:(si + 1) * 64], lhsT=kT2[hs, s0 + si, :], rhs=kT2[hs, s0 + si, :], start=True, stop=True)
                                    ins = h.matmul(pkq[hs, si * 64:(si + 1) * 64], lhsT=kT2[hs, s0 + si, :], rhs=qT2[hs, s0 + si, :], start=True, stop=True)
                            return ins
                        P.op("pe", mm_kq, reads=[b_kT2, b_qT2], writes=[b_pkq] + ([b_pkk] if is_g else []))
                        pkq3 = pkq[:, :].rearrange("p (s i) -> p s i", i=64)
                        P.op("dve", lambda h, pkq3=pkq3, ss=ss: h.tensor_tensor(out=intraT[:, ss, :], in0=pkq3, in1=dTi[:], op=ALU.mult), reads=[b_pkq, b_dTi], writes=[b_intraT])
                        if not is_g:
                            continue
                        pkk3 = pkk[:, :].rearrange("p (s i) -> p s i", i=64)
                        P.op("pool", lambda h: h.tensor_tensor(out=dTs[:], in0=dTi[:], in1=maskS[:].unsqueeze(1).to_broadcast([128, 8, 64]), op=ALU.mult),
                             reads=[b_dTi, b_maskS], writes=[b_dTs])
                        P.op("pool", lambda h, ss=ss, hd=hd: h.tensor_tensor(out=dTs[:], in0=dTs[:], in1=beta[:, hd, ss].unsqueeze(2).to_broadcast([128, 8, 64]), op=ALU.mult),
                             reads=[b_dTs, b_beta], writes=[b_dTs])
                        P.op("dve", lambda h, pkk3=pkk3: h.tensor_tensor(out=NTg[:], in0=pkk3, in1=dTs[:], op=ALU.mult), reads=[b_pkk, b_dTs], writes=[b_NT])
                        for lv in range(6):
                            P.op("pool", lambda h, lv=lv: h.tensor_tensor(out=ET[:, :, lv, :], in0=NTg[:], in1=mtl[:, lv, :].unsqueeze(1).to_broadcast([128, 8, 64]), op=ALU.mult),
                                 reads=[b_NT, b_mtl], writes=[b_ET])
                        V, b_V = Vs[0]
                        W, b_W = Ws[0]
                        i2b = ident2b[:].unsqueeze(1).to_broadcast([128, 8, 64])
                        P.op("dve", lambda h, V=V: h.tensor_tensor(out=V[:], in0=i2b, in1=ET[:, :, 0, :], op=ALU.subtract), reads=[b_ident2b, b_ET], writes=[b_V])
                        pf, b_pf = nextps()

                        def mm_f0(h, pf=pf):
                            ins = None
                            for si in range(8):
                                for hf in range(2):
                                    hs = slice(hf * 64, hf * 64 + 64)
                                    ins = h.matmul(pf[hs, si * 64:(si + 1) * 64], lhsT=ET[hs, si, 0, :], rhs=identb[hs, hs], start=True, stop=True)
                            return ins
                        P.op("pe", mm_f0, reads=[b_ET, b_identb], writes=[b_pf])
                        P.op("dve", lambda h, W=W, pf=pf: h.tensor_tensor(out=W[:], in0=i2b, in1=pf[:, :].rearrange("p (s i) -> p s i", i=64), op=ALU.subtract),
                             reads=[b_ident2b, b_pf], writes=[b_W])
                        cur = 0
                        for lv in range(1, 6):
                            V, b_V = Vs[cur]
                            W, b_W = Ws[cur]
                            Vn, b_Vn = Vs[1 - cur]
                            Wn, b_Wn = Ws[1 - cur]
                            pf, b_pf = nextps()

                            def mm_f(h, pf=pf, lv=lv, W=W):
                                ins = None
                                for si in range(8):
                                    for hf in range(2):
                                        hs = slice(hf * 64, hf * 64 + 64)
                                        ins = h.matmul(pf[hs, si * 64:(si + 1) * 64], lhsT=ET[hs, si, lv, :], rhs=W[hs, si, :], start=True, stop=True)
                                return ins
                            P.op("pe", mm_f, reads=[b_ET, b_W], writes=[b_pf])
                            P.op("act", lambda h, pf=pf: h.copy(out=Fb[:].rearrange("p s i -> p (s i)"), in_=pf[:, :]), reads=[b_pf], writes=[b_Fb])
                            last = (lv == 5)
                            pgt, b_pgt = nextps()
                            if not last:
                                pg_, b_pg_ = nextps()

                            def mm_g(h, pgt=pgt, V=V, last=last, pg_=(None if last else pg_)):
                                ins = None
                                for si in range(8):
                                    for hf in range(2):
                                        hs = slice(hf * 64, hf * 64 + 64)
                                        if not last:
                                            h.matmul(pg_[hs, si * 64:(si + 1) * 64], lhsT=V[hs, si, :], rhs=Fb[hs, si, :], start=True, stop=True)
                                        ins = h.matmul(pgt[hs, si * 64:(si + 1) * 64], lhsT=Fb[hs, si, :], rhs=V[hs, si, :], start=True, stop=True)
                                return ins
                            P.op("pe", mm_g, reads=[b_V, b_Fb], writes=[b_pgt] + ([] if last else [b_pg_]))
                            if last:
                                P.op("dve", lambda h, V=V, pgt=pgt, ss=ss: h.tensor_tensor(out=Vfin[:, ss, :], in0=V[:], in1=pgt[:, :].rearrange("p (s i) -> p s i", i=64), op=ALU.subtract),
                                     reads=[b_V, b_pgt], writes=[b_Vfin])
                            else:
                                P.op("dve", lambda h, V=V, Vn=Vn, pgt=pgt: h.tensor_tensor(out=Vn[:], in0=V[:], in1=pgt[:, :].rearrange("p (s i) -> p s i", i=64), op=ALU.subtract),
                                     reads=[b_V, b_pgt], writes=[b_Vn])
                                P.op("dve", lambda h, W=W, Wn=Wn, pg_=pg_: h.tensor_tensor(out=Wn[:], in0=W[:], in1=pg_[:, :].rearrange("p (s i) -> p s i", i=64), op=ALU.subtract),
                                     reads=[b_W, b_pg_], writes=[b_Wn])
                            cur = 1 - cur
                        for hh in range(2):
                            psol, b_psol = nextps()

                            def mm_sol(h, psol=psol, hh=hh, s0=s0):
                                ins = None
                                for si in range(4):
                                    s = s0 + hh * 4 + si
                                    for hf in range(2):
                                        hs = slice(hf * 64, hf * 64 + 64)
                                        ins = h.matmul(psol[hs, si * 128:(si + 1) * 128], lhsT=Vfin[hs, s, :], rhs=Rt[hs, s, :], start=True, stop=True)
                                return ins
                            P.op("pe", mm_sol, reads=[b_Vfin, b_R], writes=[b_psol])
                            ps3 = psol[:, :].rearrange("p (s c) -> p s c", c=128)
                            s4s = slice(s0 + hh * 4, s0 + hh * 4 + 4)
                            bb = beta[:, hd, s4s].unsqueeze(2).to_broadcast([128, 4, 64])
                            P.op("dve", lambda h, ps3=ps3, s4s=s4s, bb=bb: h.tensor_tensor(out=ut[:, s4s, :], in0=ps3[:, :, 0:64], in1=bb, op=ALU.mult), reads=[b_psol, b_beta], writes=[b_u])
                            P.op("dve", lambda h, ps3=ps3, s4s=s4s, bb=bb: h.tensor_tensor(out=wt_[:, s4s, :], in0=ps3[:, :, 64:128], in1=bb, op=ALU.mult), reads=[b_psol, b_beta], writes=[b_w])
                        pwt, b_pwt = nextps()

                        def mm_wt(h, pwt=pwt, s0=s0):
                            ins = None
                            for si in range(8):
                                for hf in range(2):
                                    hs = slice(hf * 64, hf * 64 + 64)
                                    ins = h.matmul(pwt[hs, si * 64:(si + 1) * 64], lhsT=wt_[hs, s0 + si, :], rhs=identb[hs, hs], start=True, stop=True)
                            return ins
                        P.op("pe", mm_wt, reads=[b_w, b_identb], writes=[b_pwt])
                        P.op("act", lambda h, pwt=pwt, ss=ss: h.copy(out=wT[:, ss, :], in_=pwt[:, :].rearrange("p (s i) -> p s i", i=64)), reads=[b_pwt], writes=[b_wT])

                    if K.mxstop == "g1":
                        o = nc.dram_tensor("g1_o", [128, 6, NST * 64], F32, kind="ExternalOutput").ap()
                        bo = Buf("g1_o")
                        dbg, b_dbg = osum, b_osum
                        lst = [(kd, b_kd), (intraT, b_intraT)] + ([(Vfin, b_Vfin), (ut, b_u), (wT, b_wT), (Rt, b_R)] if is_g else [])
                        for i_, (tt_, bb_) in enumerate(lst):
                            src_ = tt_[:, :, 0:64] if tt_ is Rt else tt_[:]
                            P.op("dve", lambda h, src_=src_: h.tensor_copy(out=dbg[:].rearrange("p (s i) -> p s i", i=64), in_=src_), reads=[bb_], writes=[b_dbg])
                            P.dma("sp", [(o[:, i_, :], dbg[:])], owner=b_dbg, reads=[b_dbg], writes=[bo])
                        K.outs["g1_o"] = bo
                        P.barrier()
                        return
                    P.op("dve", lambda h: h.memset(St[:], 0.0), writes=[b_S])
                    P.op("dve", lambda h: h.memset(Sb[:], 0.0), writes=[b_Sb])
                    for s in range(NST):
                        pq, b_pq = nextps()
                        if is_g:
                            pa, b_pa = nextps()

                        def mm_a(h, s=s, pq=pq, pa=(pa if is_g else None)):
                            ins = None
                            for hf in range(2):
                                hs = slice(hf * 64, hf * 64 + 64)
                                if is_g:
                                    h.matmul(pa[hs, 0:DV], lhsT=wT[hs, s, :], rhs=Sb[hs, :], start=True, stop=True)
                                ins = h.matmul(pq[hs, 0:DV], lhsT=qT2[hs, s, :], rhs=Sb[hs, :], start=True, stop=True)
                            return ins
                        P.op("pe", mm_a, reads=[b_qT2, b_Sb] + ([b_wT] if is_g else []), writes=[b_pq] + ([b_pa] if is_g else []))
                        if is_g:
                            P.op("dve", lambda h, s=s, pa=pa: h.tensor_tensor(out=vnew[:], in0=ut[:, s, :], in1=pa[:, 0:DV], op=ALU.subtract), reads=[b_u, b_pa], writes=[b_vnew])
                            vn_ap = lambda hs: vnew[hs, :]
                            vn_b = b_vnew
                        else:
                            vn_ap = lambda hs, s=s: Rt[hs, s, 0:DV]
                            vn_b = b_R
                        psn, b_psn = nextps()
                        pin, b_pin = nextps()

                        def mm_c(h, s=s, psn=psn, pin=pin, vn_ap=vn_ap):
                            ins = None
                            for hf in range(2):
                                hs = slice(hf * 64, hf * 64 + 64)
                                h.matmul(psn[hs, 0:DV], lhsT=kd[hs, s, :], rhs=vn_ap(hs), start=True, stop=True)
                                ins = h.matmul(pin[hs, 0:DV], lhsT=intraT[hs, s, :], rhs=vn_ap(hs), start=True, stop=True)
                            return ins
                        P.op("pe", mm_c, reads=[b_kd, b_intraT, vn_b], writes=[b_psn, b_pin])
                        P.op("dve", lambda h, s=s, psn=psn, hd=hd: h.scalar_tensor_tensor(out=St[:], in0=St[:], scalar=cd[:, hd, s:s + 1], in1=psn[:, 0:DV], op0=ALU.mult, op1=ALU.add),
                             reads=[b_S, b_cd, b_psn], writes=[b_S])
                        P.op("dve", lambda h: h.tensor_copy(out=Sb[:], in_=St[:]), reads=[b_S], writes=[b_Sb])
                        P.op("act", lambda h, s=s, pq=pq, hd=hd: h.activation(out=tq[:], in_=pq[:, 0:DV], func=AF.Copy, scale=egc[:, hd, s:s + 1]), reads=[b_pq, b_egc], writes=[b_tq])
                        P.op("pool" if False else "dve", lambda h, s=s, pin=pin: h.tensor_tensor(out=Ot[:, s, :], in0=tq[:], in1=pin[:, 0:DV], op=ALU.add), reads=[b_tq, b_pin], writes=[b_O])
                    if not is_g:
                        P.op("act", lambda h: h.activation(out=den[:], in_=Ot[:, :, 64:65], func=AF.Abs), reads=[b_O], writes=[b_den])
                        P.op("dve", lambda h: h.tensor_scalar_max(out=den[:], in0=den[:], scalar1=1.0), reads=[b_den], writes=[b_den])
                        P.op("dve", lambda h: h.reciprocal(out=den[:], in_=den[:]), reads=[b_den], writes=[b_den])
                        P.op("dve", lambda h: h.tensor_tensor(out=Ot[:, :, 0:64], in0=Ot[:, :, 0:64], in1=den[:].to_broadcast([128, NST, 64]), op=ALU.mult), reads=[b_O, b_den], writes=[b_O])
                    if is_g:
                        Osrc, b_Osrc = Ot, b_O
                    else:
                        P.op("act", lambda h: h.copy(out=Rt[:, :, 0:64], in_=Ot[:, :, 0:64]), reads=[b_O], writes=[b_R])
                        Osrc, b_Osrc = Rt, b_R
                    if K.mxstop == "scan":
                        o = nc.dram_tensor("scan_o", [128, NST * 64], F32, kind="ExternalOutput").ap()
                        bo = Buf("scan_o")
                        P.op("dve", lambda h: h.tensor_copy(out=osum[:].rearrange("p (s i) -> p s i", i=64), in_=Ot[:, :, 0:64]), reads=[b_O], writes=[b_osum])
                        P.dma("sp", [(o, osum[:])], owner=b_osum, reads=[b_osum], writes=[bo])
                        K.outs["scan_o"] = bo
                        P.barrier()
                        return
                    for cg in range(8):
                        pcf, b_pcf = nextps()
                        pcb, b_pcb = nextps()

                        def mm_o(h, pcf=pcf, pcb=pcb, cg=cg, sub=sub, Osrc=Osrc):
                            ins = None
                            osl = slice(sub * 64, sub * 64 + 64)
                            for ci_ in range(8):
                                n = cg * 8 + ci_
                                h.matmul(pcf[osl, ci_ * 64:(ci_ + 1) * 64], lhsT=Osrc[0:64, n, 0:64], rhs=identb[0:64, 0:64], start=True, stop=True,
                                         tile_position=(0, sub * 64))
                                ins = h.matmul(pcb[osl, ci_ * 64:(ci_ + 1) * 64], lhsT=Osrc[64:128, 63 - n, 0:64], rhs=identb[64:128, 64:128], start=True, stop=True,
                                               tile_position=(64, sub * 64))
                            return ins
                        P.op("pe", mm_o, reads=[b_Osrc, b_identb], writes=[b_pcf, b_pcb])
                        osl_ = slice(sub * 64, sub * 64 + 64)
                        csl_ = slice(cg * 512, (cg + 1) * 512)
                        P.op("act", lambda h, pcf=pcf, osl_=osl_, csl_=csl_: h.copy(out=osum[osl_, csl_], in_=pcf[osl_, :]), reads=[b_pcf], writes=[b_osum])
                        P.op("dve", lambda h, pcb=pcb, osl_=osl_, csl_=csl_: h.tensor_tensor(out=osum[osl_, csl_], in0=osum[osl_, csl_], in1=pcb[osl_, :], op=ALU.add), reads=[b_pcb, b_osum], writes=[b_osum])
                    if sub == 1:
                        zsrc = S.projT[(12 if is_g else 28) + pair]
                        P.dma("sp", [(zt[:], zsrc)], owner=b_zt, reads=[B.projT], writes=[b_zt])
                        if is_g:
                            P.op("act", lambda h: h.activation(out=sqp[:], in_=osum[:], func=AF.Square), reads=[b_osum], writes=[b_sqp])
                            P.op("act", lambda h: h.activation(out=zt[:], in_=zt[:], func=AF.Silu), reads=[b_zt], writes=[b_zt])
                            for blk in range(8):
                                sl = slice(blk * 512, (blk + 1) * 512)
                                pp, b_pp = nextps()
                                P.op("pe", lambda h, pp=pp, sl=sl: h.matmul(pp[:, :], lhsT=bones[:], rhs=sqp[:, sl], start=True, stop=True), reads=[b_bones, b_sqp], writes=[b_pp])
                                P.op("act", lambda h, pp=pp: h.activation(out=rnp[:], in_=pp[:, :], func=AF.Sqrt, bias=eps_t[:], scale=1.0 / 64), reads=[b_pp, b_eps], writes=[b_rnp])
                                P.op("dve", lambda h: h.reciprocal(out=rnp[:], in_=rnp[:]), reads=[b_rnp], writes=[b_rnp])
                                P.op("dve", lambda h, sl=sl: h.scalar_tensor_tensor(out=osum[:, sl], in0=osum[:, sl], scalar=gng[:, 0:1], in1=rnp[:], op0=ALU.mult, op1=ALU.mult),
                                     reads=[b_osum, b_gng, b_rnp], writes=[b_osum])
                                P.op("dve", lambda h, sl=sl: h.tensor_tensor(out=oout[:, sl], in0=osum[:, sl], in1=zt[:, sl], op=ALU.mult), reads=[b_osum, b_zt], writes=[b_oout])
                        else:
                            P.op("act", lambda h: h.activation(out=zt[:], in_=zt[:], func=AF.Sigmoid), reads=[b_zt], writes=[b_zt])
                            P.op("act", lambda h: h.copy(out=obf[:], in_=osum[:]), reads=[b_osum], writes=[b_obf])
                            for blk in range(8):
                                sl = slice(blk * 512, (blk + 1) * 512)
                                pm_, b_pm_ = nextps()
                                P.op("pe", lambda h, pm_=pm_, sl=sl: h.matmul(pm_[:, :], lhsT=bones[:], rhs=obf[:, sl], start=True, stop=True), reads=[b_bones, b_obf], writes=[b_pm_])
                                P.op("act", lambda h, pm_=pm_: h.activation(out=mu[:], in_=pm_[:, :], func=AF.Copy, scale=1.0 / 64), reads=[b_pm_], writes=[b_mu])
                                P.op("dve", lambda h, sl=sl: h.tensor_tensor(out=osum[:, sl], in0=osum[:, sl], in1=mu[:], op=ALU.subtract), reads=[b_osum, b_mu], writes=[b_osum])
                                P.op("act", lambda h, sl=sl: h.activation(out=sqp[:, sl], in_=osum[:, sl], func=AF.Square), reads=[b_osum], writes=[b_sqp])
                                pp, b_pp = nextps()
                                P.op("pe", lambda h, pp=pp, sl=sl: h.matmul(pp[:, :], lhsT=bones[:], rhs=sqp[:, sl], start=True, stop=True), reads=[b_bones, b_sqp], writes=[b_pp])
                                P.op("act", lambda h, pp=pp: h.activation(out=rnp[:], in_=pp[:, :], func=AF.Sqrt, bias=eps_t[:], scale=1.0 / 64), reads=[b_pp, b_eps], writes=[b_rnp])
                                P.op("dve", lambda h: h.reciprocal(out=rnp[:], in_=rnp[:]), reads=[b_rnp], writes=[b_rnp])
                                P.op("dve", lambda h, sl=sl, pair=pair: h.scalar_tensor_tensor(out=osum[:, sl], in0=osum[:, sl], scalar=gng[:, pair:pair + 1], in1=rnp[:], op0=ALU.mult, op1=ALU.mult),
                                     reads=[b_osum, b_gng, b_rnp], writes=[b_osum])
                                P.op("dve", lambda h, sl=sl: h.tensor_tensor(out=oout[:, sl], in0=osum[:, sl], in1=zt[:, sl], op=ALU.mult), reads=[b_osum, b_zt], writes=[b_oout])
                        dst = (S.oaT if is_g else S.hbT)[pair]
                        P.dma("sp", [(dst, oout[:])], owner=b_oout, reads=[b_oout], writes=[B.oaT if is_g else B.hbT])
            P.barrier()

        ones_full, b_onesfull = sb("ones_full", [128, 128], F32)
        P.op("pool", lambda h: h.memset(ones_full[:], 1.0), writes=[b_onesfull])

        def row_bcast(dst, b_dst, col_ap, b_col, stack):
            dgt, b_dgt = sb("rb_dg", [128, 8, 128], F32, stack)
            for k in range(8):
                P.op("dve", lambda h, k=k: h.tensor_scalar(out=dgt[:, k, :], in0=ident[:], scalar1=col_ap[:, k:k + 1], scalar2=None, op0=ALU.mult),
                     reads=[b_ident, b_col], writes=[b_dgt])
            for hf in range(2):
                pb, b_pb = nextps()
                P.op("pe", lambda h, pb=pb, hf=hf: h.matmul(pb[:, :], lhsT=ones_full[:], rhs=dgt[:, hf * 4:(hf + 1) * 4, :].rearrange("p k n -> p (k n)"), start=True, stop=True),
                     reads=[b_onesfull, b_dgt], writes=[b_pb])
                P.op("act", lambda h, pb=pb, hf=hf: h.copy(out=dst[:, hf * 512:(hf + 1) * 512], in_=pb[:, :]), reads=[b_pb], writes=[b_dst])

        def stage_out(l, h_src, b_hsrc, h_dst, b_hdst, gt_col, b_gt):
            with ExitStack() as s5:
                gtb, b_gtb = sb("o_gtb", [128, D], F32, s5)
                wm, b_wm = sb("o_wm", [128, 8, 2048], BF16, s5)
                wba, b_wba = sb("o_wba", [128, 4, D], BF16, s5)
                wbb, b_wbb = sb("o_wbb", [128, 4, D], BF16, s5)
                wo, b_wo = sb("o_wo", [128, 8, D], BF16, s5)
                wsrc = I.w_in[l].rearrange("(k p) n -> p k n", p=128)
                for q4 in range(4):
                    P.dma("pool", [(wm[:, :, q4 * 512:(q4 + 1) * 512], wsrc[:, :, C_MERGE + q4 * 512:C_MERGE + (q4 + 1) * 512])], owner=b_wm, reads=[B.w], writes=[b_wm])
                P.dma("pool", [(wba[:], I.w_branch_a[l].rearrange("(k p) n -> p k n", p=128))], owner=b_wba, reads=[B.w], writes=[b_wba])
                P.dma("pool", [(wbb[:], I.w_branch_b[l].rearrange("(k p) n -> p k n", p=128))], owner=b_wbb, reads=[B.w], writes=[b_wbb])
                with ExitStack() as s5a:
                    row_bcast(gtb, b_gtb, gt_col, b_gt, s5a)
                    wst, b_wst = sb("o_wst", [128, 8, D], F32, s5a)
                    wos = I.w_out[l].rearrange("(k p) n -> p k n", p=128)
                    P.dma("sp", [(wst[:, 0:4, :], wos[:, 0:4, :]), (wst[:, 4:8, :], wos[:, 4:8, :])], owner=b_wst, reads=[B.w], writes=[b_wst])
                    for k in range(8):
                        P.op("dve" if k % 2 else "pool", lambda h, k=k: h.tensor_tensor(out=wo[:, k, :], in0=wst[:, k, :], in1=gtb[:], op=ALU.mult), reads=[b_wst, b_gtb], writes=[b_wo])
                    P.barrier()
                hns = [sb("o_hn%d" % i, [128, 8, 512], BF16, s5) for i in range(2)]
                oas = [sb("o_oa%d" % i, [128, 4, 512], BF16, s5) for i in range(2)]
                hbs = [sb("o_hb%d" % i, [128, 4, 512], BF16, s5) for i in range(2)]
                yT, b_yT = sb("o_yT", [128, 8, 512], BF16, s5)
                gas = [sb("o_ga%d" % i, [128, 512], F32, s5) for i in range(2)]
                gbs = [sb("o_gb%d" % i, [128, 512], F32, s5) for i in range(2)]
                y1s = [sb("o_y1%d" % i, [128, 512], F32, s5) for i in range(2)]
                hts = [sb("o_ht%d" % i, [128, D], F32, s5) for i in range(2)]
                for tb in range(8):
                    tsl = slice(tb * 512, (tb + 1) * 512)
                    hn, b_hn = hns[tb % 2]
                    oa, b_oa = oas[tb % 2]
                    hb, b_hb = hbs[tb % 2]
                    P.dma("sp", [(hn[:, k, :], S.hnT[k][:, tsl]) for k in range(8)], owner=b_hn, reads=[B.hnT], writes=[b_hn])
                    P.dma("sp", [(oa[:, k, :], S.oaT[k][:, tsl]) for k in range(4)], owner=b_oa, reads=[B.oaT], writes=[b_oa])
                    P.dma("sp", [(hb[:, k, :], S.hbT[k][:, tsl]) for k in range(4)], owner=b_hb, reads=[B.hbT], writes=[b_hb])
                    for dc in range(8):
                        dsl = slice(dc * 128, (dc + 1) * 128)
                        ga, b_ga = gas[dc % 2]
                        gb, b_gb = gbs[dc % 2]
                        y1, b_y1 = y1s[dc % 2]
                        pga, b_pga = nextps()
                        pgb, b_pgb = nextps()
                        pa, b_pa = nextps()
                        pb, b_pb = nextps()

                        def mm(h, pga=pga, pgb=pgb, pa=pa, pb=pb, hn=hn, oa=oa, hb=hb, dc=dc):
                            ins = None
                            for k in range(8):
                                h.matmul(pga[:, :], lhsT=wm[:, k, dc * 128:(dc + 1) * 128], rhs=hn[:, k, :], start=(k == 0), stop=(k == 7))
                            for k in range(8):
                                h.matmul(pgb[:, :], lhsT=wm[:, k, 1024 + dc * 128:1024 + (dc + 1) * 128], rhs=hn[:, k, :], start=(k == 0), stop=(k == 7))
                            for k in range(4):
                                h.matmul(pa[:, :], lhsT=wba[:, k, dc * 128:(dc + 1) * 128], rhs=oa[:, k, :], start=(k == 0), stop=(k == 3))
                            for k in range(4):
                                ins = h.matmul(pb[:, :], lhsT=wbb[:, k, dc * 128:(dc + 1) * 128], rhs=hb[:, k, :], start=(k == 0), stop=(k == 3))
                            return ins
                        P.op("pe", mm, reads=[b_wm, b_wba, b_wbb, b_hn, b_oa, b_hb], writes=[b_pga, b_pgb, b_pa, b_pb])
                        P.op("act", lambda h, ga=ga, pga=pga: h.activation(out=ga[:], in_=pga[:, :], func=AF.Sigmoid), reads=[b_pga], writes=[b_ga])
                        P.op("act", lambda h, gb=gb, pgb=pgb: h.activation(out=gb[:], in_=pgb[:, :], func=AF.Sigmoid), reads=[b_pgb], writes=[b_gb])
                        P.op("dve", lambda h, y1=y1, ga=ga, pa=pa: h.tensor_tensor(out=y1[:], in0=ga[:], in1=pa[:, :], op=ALU.mult), reads=[b_ga, b_pa], writes=[b_y1])
                        P.op("dve", lambda h, gb=gb, pb=pb: h.tensor_tensor(out=gb[:], in0=gb[:], in1=pb[:, :], op=ALU.mult), reads=[b_gb, b_pb], writes=[b_gb])
                        P.op("pool", lambda h, y1=y1, gb=gb, dc=dc: h.tensor_tensor(out=yT[:, dc, :], in0=y1[:], in1=gb[:], op=ALU.add), reads=[b_y1, b_gb], writes=[b_yT])
                    for tt in range(4):
                        t = tb * 4 + tt
                        ht, b_ht = hts[t % 2]
                        P.dma("sp", [(ht[:], h_src[t * 128:(t + 1) * 128, :])], owner=b_ht, reads=[b_hsrc], writes=[b_ht])
                        for hf in range(2):
                            po, b_po = nextps()

                            def mmo(h, po=po, tt=tt, hf=hf):
                                ins = None
                                for k in range(8):
                                    ins = h.matmul(po[:, :], lhsT=yT[:, k, tt * 128:(tt + 1) * 128], rhs=wo[:, k, hf * 512:(hf + 1) * 512], start=(k == 0), stop=(k == 7))
                                return ins
                            P.op("pe", mmo, reads=[b_yT, b_wo], writes=[b_po])
                            P.op("dve", lambda h, ht=ht, po=po, hf=hf: h.tensor_tensor(out=ht[:, hf * 512:(hf + 1) * 512], in0=ht[:, hf * 512:(hf + 1) * 512], in1=po[:, :], op=ALU.add),
                                 reads=[b_ht, b_po], writes=[b_ht])
                        P.dma("sp", [(h_dst[t * 128:(t + 1) * 128, :], ht[:])], owner=b_ht, reads=[b_ht], writes=[b_hdst])
            P.barrier()

        def stage_norm_router(l, h_src, b_hsrc, gs_ap, sh_ap, b_par):
            with ExitStack() as s6:
                xts = [sb("r_xt%d" % i, [128, D], F32, s6) for i in range(2)]
                xss = [sb("r_xs%d" % i, [128, D], F32, s6) for i in range(2)]
                sts = [sb("r_st%d" % i, [128, 4], F32, s6) for i in range(2)]
                hts = [sb("r_ht%d" % i, [128, 8, 512], BF16, s6) for i in range(2)]
                hfs = [sb("r_hf%d" % i, [128, 8, 128], F32, s6) for i in range(2)]
                junk, b_junk = sb("r_junk", [128, D], BF16, s6)
                wr, b_wr = sb("r_wr", [128, 8, 36], F32, s6)
                P.dma("sp", [(wr[:, :, 0:4], I.router_group[l].rearrange("(k p) n -> p k n", p=128)),
                             (wr[:, :, 4:36], I.router_expert[l].rearrange("(k p) n -> p k n", p=128))], owner=b_wr, reads=[B.w], writes=[b_wr])
                lg, b_lg = sb("r_lg", [128, NT, 36], F32, s6)
                comb, b_comb = sb("r_comb", [128, NT, 32], F32, s6)
                for t in range(NT):
                    xt, b_xt = xts[t % 2]
                    xs, b_xs = xss[t % 2]
                    stt, b_st = sts[t % 2]
                    ht, b_ht = hts[(t // 4) % 2]
                    hf_, b_hf = hfs[t % 2]
                    P.dma("sp", [(xt[:], h_src[t * 128:(t + 1) * 128, :])], owner=b_xt, reads=[b_hsrc], writes=[b_xt])
                    P.op("act", lambda h, xt=xt, stt=stt: h.activation(out=junk[:], in_=xt[:], func=AF.Square, accum_out=stt[:, 0:1]),
                         reads=[b_xt], writes=[b_junk, b_st])
                    P.op("act", lambda h, stt=stt: h.activation(out=stt[:, 1:2], in_=stt[:, 0:1], func=AF.Sqrt, scale=1.0 / D, bias=eps_t[:]),
                         reads=[b_st, b_eps], writes=[b_st])
                    P.op("dve", lambda h, stt=stt: h.reciprocal(out=stt[:, 2:3], in_=stt[:, 1:2]), reads=[b_st], writes=[b_st])
                    P.op("dve", lambda h, xs=xs, xt=xt, stt=stt: h.tensor_scalar(out=xs[:], in0=xt[:], scalar1=stt[:, 2:3], scalar2=None, op0=ALU.mult),
                         reads=[b_xt, b_st], writes=[b_xs])
                    for half in range(2):
                        pt, b_pt = nextps()

                        def tr(h, pt=pt, xs=xs, half=half):
                            ins = None
                            for q in range(4):
                                k = half * 4 + q
                                ins = h.transpose(pt[:, q * 128:(q + 1) * 128], xs[:, k * 128:(k + 1) * 128], ident[:])
                            return ins
                        P.op("pe", tr, reads=[b_xs, b_ident], writes=[b_pt])
                        for q in range(4):
                            k = half * 4 + q
                            src = pt[:, q * 128:(q + 1) * 128]
                            P.op("act", lambda h, hf_=hf_, src=src, k=k: h.activation(out=hf_[:, k, :], in_=src, func=AF.Identity, scale=gs_ap[:, k:k + 1], bias=sh_ap[:, k:k + 1]),
                                 reads=[b_pt, b_par], writes=[b_hf])
                    P.op("dve", lambda h, ht=ht, hf_=hf_, t=t: h.tensor_copy(out=ht[:, :, (t % 4) * 128:(t % 4 + 1) * 128], in_=hf_[:]), reads=[b_hf], writes=[b_ht])
                    pl, b_pl = nextps()

                    def mml(h, pl=pl, hf_=hf_):
                        ins = None
                        for k in range(8):
                            ins = h.matmul(pl[:, 0:36], lhsT=hf_[:, k, :], rhs=wr[:, k, :], start=(k == 0), stop=(k == 7))
                        return ins
                    P.op("pe", mml, reads=[b_hf, b_wr], writes=[b_pl])
                    P.op("act", lambda h, pl=pl, t=t: h.copy(out=lg[:, t, :], in_=pl[:, 0:36]), reads=[b_pl], writes=[b_lg])
                    if t % 4 == 3:
                        tb = t // 4
                        P.dma("sp", [(S.hnT[k, :, tb * 512:(tb + 1) * 512], ht[:, k, :]) for k in range(8)],
                              owner=b_ht, reads=[b_ht], writes=[B.hnT])
                def tl(name, shape):
                    return sb("r_" + name, shape, F32, s6)
                gmax, b_gmax = tl("gmax", [128, NT, 1])
                ge, b_ge = tl("ge", [128, NT, 4])
                gsum, b_gsum = tl("gsum", [128, NT, 1])
                ohg, b_ohg = tl("ohg", [128, NT, 4])
                esel, b_esel = tl("esel", [128, NT, 4, 8])
                es, b_es = tl("es", [128, NT, 8])
                m1, b_m1 = tl("m1", [128, NT, 1])
                oh1, b_oh1 = tl("oh1", [128, NT, 8])
                es2, b_es2 = tl("es2", [128, NT, 8])
                m2, b_m2 = tl("m2", [128, NT, 1])
                oh2, b_oh2 = tl("oh2", [128, NT, 8])
                w1, b_w1 = tl("w1", [128, NT, 1])
                w2, b_w2 = tl("w2", [128, NT, 1])
                glog = lg[:, :, 0:4]
                P.op("dve", lambda h: h.tensor_reduce(out=gmax[:], in_=glog, axis=AX.X, op=ALU.max), reads=[b_lg], writes=[b_gmax])
                P.op("dve", lambda h: h.tensor_tensor(out=ge[:], in0=glog, in1=gmax[:].to_broadcast([128, NT, 4]), op=ALU.subtract), reads=[b_lg, b_gmax], writes=[b_ge])
                P.op("dve", lambda h: h.tensor_scalar(out=ohg[:], in0=ge[:], scalar1=0.0, scalar2=None, op0=ALU.is_ge), reads=[b_ge], writes=[b_ohg])
                P.op("act", lambda h: h.activation(out=ge[:], in_=ge[:], func=AF.Exp), reads=[b_ge], writes=[b_ge])
                P.op("dve", lambda h: h.tensor_reduce(out=gsum[:], in_=ge[:], axis=AX.X, op=ALU.add), reads=[b_ge], writes=[b_gsum])
                P.op("dve", lambda h: h.reciprocal(out=gsum[:], in_=gsum[:]), reads=[b_gsum], writes=[b_gsum])
                el4 = lg[:, :, 4:36].rearrange("p t (g e) -> p t g e", e=8)
                P.op("dve", lambda h: h.tensor_tensor(out=esel[:], in0=el4, in1=ohg[:].unsqueeze(3).to_broadcast([128, NT, 4, 8]), op=ALU.mult), reads=[b_lg, b_ohg], writes=[b_esel])
                P.op("dve", lambda h: h.tensor_reduce(out=es[:], in_=esel[:].rearrange("p t g e -> p t e g"), axis=AX.X, op=ALU.add), reads=[b_esel], writes=[b_es])
                P.op("dve", lambda h: h.tensor_reduce(out=m1[:], in_=es[:], axis=AX.X, op=ALU.max), reads=[b_es], writes=[b_m1])
                P.op("dve", lambda h: h.tensor_tensor(out=oh1[:], in0=es[:], in1=m1[:].to_broadcast([128, NT, 8]), op=ALU.is_ge), reads=[b_es, b_m1], writes=[b_oh1])
                P.op("dve", lambda h: h.scalar_tensor_tensor(out=es2[:], in0=oh1[:], scalar=-1e30, in1=es[:], op0=ALU.mult, op1=ALU.add), reads=[b_oh1, b_es], writes=[b_es2])
                P.op("dve", lambda h: h.tensor_reduce(out=m2[:], in_=es2[:], axis=AX.X, op=ALU.max), reads=[b_es2], writes=[b_m2])
                P.op("dve", lambda h: h.tensor_tensor(out=oh2[:], in0=es2[:], in1=m2[:].to_broadcast([128, NT, 8]), op=ALU.is_ge), reads=[b_es2, b_m2], writes=[b_oh2])
                P.op("dve", lambda h: h.tensor_tensor(out=w2[:], in0=m2[:], in1=m1[:], op=ALU.subtract), reads=[b_m1, b_m2], writes=[b_w2])
                P.op("act", lambda h: h.activation(out=w2[:], in_=w2[:], func=AF.Sigmoid), reads=[b_w2], writes=[b_w2])
                P.op("dve", lambda h: h.tensor_scalar(out=w1[:], in0=w2[:], scalar1=-1.0, scalar2=1.0, op0=ALU.mult, op1=ALU.add), reads=[b_w2], writes=[b_w1])
                P.op("dve", lambda h: h.tensor_tensor(out=w1[:], in0=w1[:], in1=gsum[:], op=ALU.mult), reads=[b_w1, b_gsum], writes=[b_w1])
                P.op("dve", lambda h: h.tensor_tensor(out=w2[:], in0=w2[:], in1=gsum[:], op=ALU.mult), reads=[b_w2, b_gsum], writes=[b_w2])
                P.op("dve", lambda h: h.tensor_tensor(out=oh1[:], in0=oh1[:], in1=w1[:].to_broadcast([128, NT, 8]), op=ALU.mult), reads=[b_oh1, b_w1], writes=[b_oh1])
                P.op("dve", lambda h: h.tensor_tensor(out=oh2[:], in0=oh2[:], in1=w2[:].to_broadcast([128, NT, 8]), op=ALU.mult), reads=[b_oh2, b_w2], writes=[b_oh2])
                P.op("dve", lambda h: h.tensor_tensor(out=oh1[:], in0=oh1[:], in1=oh2[:], op=ALU.add), reads=[b_oh1, b_oh2], writes=[b_oh1])
                c4 = comb[:].rearrange("p t (g e) -> p t g e", e=8)
                P.op("dve", lambda h: h.tensor_tensor(out=c4, in0=ohg[:].unsqueeze(3).to_broadcast([128, NT, 4, 8]), in1=oh1[:].unsqueeze(2).to_broadcast([128, NT, 4, 8]), op=ALU.mult),
                     reads=[b_ohg, b_oh1], writes=[b_comb])
                P.dma("sp", [(S.comb, comb[:].rearrange("p t e -> p (t e)"))], owner=b_comb, reads=[b_comb], writes=[B.comb])
            P.barrier()

        def stage_moe(l, h_src, b_hsrc, h_dst, b_hdst, gt_col, b_gt):
            with ExitStack() as s7:
                gtb, b_gtb = sb("e_gtb", [128, D], F32, s7)
                with ExitStack() as s7a:
                    row_bcast(gtb, b_gtb, gt_col, b_gt, s7a)
                    P.barrier()
                comb, b_comb = sb("e_comb", [128, NT, 32], F32, s7)
                P.dma("sp", [(comb[:].rearrange("p t e -> p (t e)"), S.comb)], owner=b_comb, reads=[B.comb], writes=[b_comb])
                hn, b_hn = sb("e_hn", [128, 8, 2048], BF16, s7)
                yacc, b_yacc = sb("e_yacc", [128, 16, D], F32, s7)
                wgs = [sb("e_wgs%d" % i, [128, 8, 256], F32, s7) for i in range(2)]
                wus = [sb("e_wus%d" % i, [128, 8, 256], F32, s7) for i in range(2)]
                wds = [sb("e_wds%d" % i, [128, 2, D], F32, s7) for i in range(2)]
                wgb = [sb("e_wgb%d" % i, [128, 8, 256], BF16, s7) for i in range(2)]
                wub = [sb("e_wub%d" % i, [128, 8, 256], BF16, s7) for i in range(2)]
                wdb = [sb("e_wdb%d" % i, [128, 2, D], BF16, s7) for i in range(2)]
                sgs = [sb("e_sg%d" % i, [128, 512], F32, s7) for i in range(2)]
                aTs = [sb("e_aT%d" % i, [128, 2, 512], BF16, s7) for i in range(2)]
                hts = [sb("e_ht%d" % i, [128, D], F32, s7) for i in range(2)]
                it = 0
                for half in range(2):
                    P.dma("sp", [(hn[:, k, :], S.hnT[k][:, half * 2048:(half + 1) * 2048]) for k in range(8)], owner=b_hn, reads=[B.hnT], writes=[b_hn])
                    P.op("pool", lambda h: h.memset(yacc[:], 0.0), writes=[b_yacc])
                    for e in range(32):
                        i2 = e % 2
                        (wg_s, b_wgs_), (wu_s, b_wus_), (wd_s, b_wds_) = wgs[i2], wus[i2], wds[i2]
                        (wg, b_wg), (wu, b_wu), (wd, b_wd) = wgb[i2], wub[i2], wdb[i2]
                        P.dma("sp", [(wg_s[:], I.w_gate[l][e].rearrange("(k p) f -> p k f", p=128))], owner=b_wgs_, reads=[B.w], writes=[b_wgs_])
                        P.dma("sp", [(wu_s[:], I.w_up[l][e].rearrange("(k p) f -> p k f", p=128))], owner=b_wus_, reads=[B.w], writes=[b_wus_])
                        P.dma("sp", [(wd_s[:], I.w_down[l][e].rearrange("(k p) n -> p k n", p=128))], owner=b_wds_, reads=[B.w], writes=[b_wds_])
                        P.op("pool", lambda h, wg=wg, wg_s=wg_s: h.tensor_copy(out=wg[:], in_=wg_s[:]), reads=[b_wgs_], writes=[b_wg])
                        P.op("pool", lambda h, wu=wu, wu_s=wu_s: h.tensor_copy(out=wu[:], in_=wu_s[:]), reads=[b_wus_], writes=[b_wu])
                        P.op("pool", lambda h, wd=wd, wd_s=wd_s: h.tensor_copy(out=wd[:], in_=wd_s[:]), reads=[b_wds_], writes=[b_wd])
                        for tb in range(4):
                            aT, b_aT = aTs[it % 2]
                            it += 1
                            for f in range(2):
                                sg, b_sg = sgs[f]
                                pg, b_pg = nextps()
                                pu, b_pu = nextps()

                                def mmgu(h, pg=pg, pu=pu, wg=wg, wu=wu, f=f, tb=tb):
                                    ins = None
                                    for k in range(8):
                                        h.matmul(pg[:, :], lhsT=wg[:, k, f * 128:(f + 1) * 128], rhs=hn[:, k, tb * 512:(tb + 1) * 512], start=(k == 0), stop=(k == 7))
                                    for k in range(8):
                                        ins = h.matmul(pu[:, :], lhsT=wu[:, k, f * 128:(f + 1) * 128], rhs=hn[:, k, tb * 512:(tb + 1) * 512], start=(k == 0), stop=(k == 7))
                                    return ins
                                P.op("pe", mmgu, reads=[b_wg, b_wu, b_hn], writes=[b_pg, b_pu])
                                P.op("act", lambda h, sg=sg, pg=pg: h.activation(out=sg[:], in_=pg[:, :], func=AF.Silu), reads=[b_pg], writes=[b_sg])
                                P.op("dve", lambda h, aT=aT, sg=sg, pu=pu, f=f: h.tensor_tensor(out=aT[:, f, :], in0=sg[:], in1=pu[:, :], op=ALU.mult), reads=[b_sg, b_pu], writes=[b_aT])
                            for tt in range(4):
                                tl_ = tb * 4 + tt
                                tg = half * 16 + tl_
                                for hf in range(2):
                                    pd, b_pd = nextps()

                                    def mmd(h, pd=pd, aT=aT, wd=wd, tt=tt, hf=hf):
                                        ins = None
                                        for f in range(2):
                                            ins = h.matmul(pd[:, :], lhsT=aT[:, f, tt * 128:(tt + 1) * 128], rhs=wd[:, f, hf * 512:(hf + 1) * 512], start=(f == 0), stop=(f == 1))
                                        return ins
                                    P.op("pe", mmd, reads=[b_aT, b_wd], writes=[b_pd])
                                    ysl = yacc[:, tl_, hf * 512:(hf + 1) * 512]
                                    P.op("dve", lambda h, pd=pd, ysl=ysl, tg=tg, e=e: h.scalar_tensor_tensor(out=ysl, in0=pd[:, :], scalar=comb[:, tg, e:e + 1], in1=ysl, op0=ALU.mult, op1=ALU.add),
                                         reads=[b_pd, b_comb, b_yacc], writes=[b_yacc])
                    for tl_ in range(16):
                        tg = half * 16 + tl_
                        ht, b_ht = hts[tl_ % 2]
                        P.dma("sp", [(ht[:], h_src[tg * 128:(tg + 1) * 128, :])], owner=b_ht, reads=[b_hsrc], writes=[b_ht])
                        P.op("pool", lambda h, tl_=tl_: h.tensor_tensor(out=yacc[:, tl_, :], in0=yacc[:, tl_, :], in1=gtb[:], op=ALU.mult), reads=[b_yacc, b_gtb], writes=[b_yacc])
                        P.op("dve", lambda h, ht=ht, tl_=tl_: h.tensor_tensor(out=ht[:], in0=ht[:], in1=yacc[:, tl_, :], op=ALU.add), reads=[b_ht, b_yacc], writes=[b_ht])
                        P.dma("sp", [(h_dst[tg * 128:(tg + 1) * 128, :], ht[:])], owner=b_ht, reads=[b_ht], writes=[b_hdst])
            P.barrier()

        def stage_final(h_src, b_hsrc):
            with ExitStack() as s8:
                gf, b_gf = sb("f_g", [128, D], F32, s8)
                P.dma("sp", [(gf[:], I.gfin_b)], owner=b_gf, writes=[b_gf])
                xts = [sb("f_xt%d" % i, [128, D], F32, s8) for i in range(2)]
                sts = [sb("f_st%d" % i, [128, 4], F32, s8) for i in range(2)]
                junk, b_junk = sb("f_junk", [128, D], BF16, s8)
                for t in range(NT):
                    xt, b_xt = xts[t % 2]
                    stt, b_st = sts[t % 2]
                    P.dma("sp", [(xt[:], h_src[t * 128:(t + 1) * 128, :])], owner=b_xt, reads=[b_hsrc], writes=[b_xt])
                    P.op("act", lambda h, xt=xt, stt=stt: h.activation(out=junk[:], in_=xt[:], func=AF.Square, accum_out=stt[:, 0:1]), reads=[b_xt], writes=[b_junk, b_st])
                    P.op("act", lambda h, stt=stt: h.activation(out=stt[:, 1:2], in_=stt[:, 0:1], func=AF.Sqrt, scale=1.0 / D, bias=eps_t[:]), reads=[b_st, b_eps], writes=[b_st])
                    P.op("dve", lambda h, stt=stt: h.reciprocal(out=stt[:, 2:3], in_=stt[:, 1:2]), reads=[b_st], writes=[b_st])
                    P.op("dve", lambda h, xt=xt, stt=stt: h.scalar_tensor_tensor(out=xt[:], in0=xt[:], scalar=stt[:, 2:3], in1=gf[:], op0=ALU.mult, op1=ALU.mult), reads=[b_xt, b_st, b_gf], writes=[b_xt])
                    P.dma("sp", [(out[t * 128:(t + 1) * 128, :], xt[:])], owner=b_xt, reads=[b_xt], writes=[B.out])
            P.barrier()

        h_cur, b_hcur = I.x, B.x
        if layers is None:
            layers = list(range(depth))
        for l in layers:
            mt, b_mt = modT[l]
            par, b_par = sb("par%d" % l, [128, 4, 8], F32)
            P.op("dve", lambda h, par=par, mt=mt, l=l: h.scalar_tensor_tensor(out=par[:, 0, :], in0=mt[:, 8:16], scalar=1.0, in1=gmix[:, l, :], op0=ALU.add, op1=ALU.mult),
                 reads=[b_mt, b_gmix], writes=[b_par])
            P.op("dve", lambda h, par=par, mt=mt, l=l: h.scalar_tensor_tensor(out=par[:, 1, :], in0=mt[:, 32:40], scalar=1.0, in1=gffn[:, l, :], op0=ALU.add, op1=ALU.mult),
                 reads=[b_mt, b_gffn], writes=[b_par])
            stage_norm(h_cur, b_hcur, par[:, 0, :], mt[:, 0:8], b_par, S.hnT, B.hnT, "a%d" % l)
            if stop_after == "norm1" and l == stop_layer:
                break
            stage_proj(l)
            if stop_after == "proj" and l == stop_layer:
                break
            stage_prep(l)
            if stop_after == "prep" and l == stop_layer:
                break
            stage_mixer(l, "gdn")
            if stop_after == "gdn" and l == stop_layer:
                break
            stage_mixer(l, "ml")
            if stop_after == "ml" and l == stop_layer:
                break
            bA = getattr(B, "hA%d" % l)
            bB = getattr(B, "hB%d" % l)
            stage_out(l, h_cur, b_hcur, S.hA[l], bA, mt[:, 16:24], b_mt)
            if stop_after == "out" and l == stop_layer:
                break
            stage_norm_router(l, S.hA[l], bA, par[:, 1, :], mt[:, 24:32], b_par)
            if stop_after == "router" and l == stop_layer:
                break
            if out_h and l == layers[-1]:
                stage_moe(l, S.hA[l], bA, out, B.out, mt[:, 40:48], b_mt)
                h_cur, b_hcur = out, B.out
            else:
                stage_moe(l, S.hA[l], bA, S.hB[l], bB, mt[:, 40:48], b_mt)
                h_cur, b_hcur = S.hB[l], bB
            if stop_after == "moe" and l == stop_layer:
                break
        if stop_after == "all" and do_final:
            stage_final(h_cur, b_hcur)

        finals = [b for b in (B.hnT, B.projT, B.gcol, B.prepT, B.oaT, B.hbT, B.comb, B.hA0, B.hA1, B.hB0, B.hB1, B.out) if b.lw is not None] + list(K.outs.values())
        P.finish(finals)
    return nc

def col(v):
    return np.ascontiguousarray(v.reshape(-1, 128).T)
def shared_inputs(inp):
    L = 2
    convw = np.zeros((L, 128, 20, 5), np.float32)
    gpar = np.zeros((L, 128, 4, 8), np.float32)
    gng = np.zeros((L, 2, 128, 4), np.float32)
    for l in range(L):
        convw[l, :, 0:12, :] = inp["gdn_conv_w"][l].T.reshape(12, 128, 5).transpose(1, 0, 2)
        convw[l, :, 12:20, :] = inp["mlstm_conv_w"][l].T.reshape(8, 128, 5).transpose(1, 0, 2)
        for i, nm in enumerate(["gdn_dt_bias", "gdn_a_log", "mlstm_i_bias", "mlstm_f_bias"]):
            gpar[l, 0:64, i, :] = inp[nm][l][0][None, :]
            gpar[l, 64:128, i, :] = inp[nm][l][1][None, :]
        gng[l, 0, :, 0] = np.tile(inp["gdn_norm_g"][l], 2)
        gng[l, 1] = inp["mlstm_norm_g"][l].reshape(4, 128).T
    m = {
     "ada_w": inp["ada_w"],
     "ada_b": np.ascontiguousarray(inp["ada_b"].reshape(2, 48, 128).transpose(0, 2, 1)),
     "gmix": np.stack([col(inp["norm_mix_g"][l]) for l in range(2)]),
     "gffn": np.stack([col(inp["norm_ffn_g"][l]) for l in range(2)]),
     "gfin": col(inp["final_norm_g"]), "w_in": inp["w_in"],
     "convw": convw, "gpar": gpar, "gng": gng,
     "w_branch_a": inp["w_branch_a"], "w_branch_b": inp["w_branch_b"], "w_out": inp["w_out"],
     "router_group": inp["router_group"], "router_expert": inp["router_expert"],
     "w_gate": inp["w_gate"], "w_up": inp["w_up"], "w_down": inp["w_down"],
     "gfin_b": np.ascontiguousarray(np.broadcast_to(inp["final_norm_g"][None, :], (128, 1024))),
    }
    return m
def core_inputs(inp, b):
    return {"x": np.ascontiguousarray(inp["x"][b]), "c_col": col(inp["c"][b])}


_NC_CACHE = {}
FUSED = False


def kernel(**inputs):
    from concourse.bass_utils import run_bass_kernel_spmd
    inp = {k: np.asarray(v) for k, v in inputs.items()}
    shared = shared_inputs(inp)
    in_maps = []
    for b in range(8):
        m = dict(shared)
        m.update(core_inputs(inp, b))
        in_maps.append(m)
    if FUSED:
        if "nc" not in _NC_CACHE:
            _NC_CACHE["nc"] = build()
        res = run_bass_kernel_spmd(_NC_CACHE["nc"], in_maps, core_ids=list(range(8)))
    else:
        if "nc0" not in _NC_CACHE:
            _NC_CACHE["nc0"] = build(layers=[0], do_final=False, out_h=True)
            _NC_CACHE["nc1"] = build(layers=[1], do_final=True)
        r0 = run_bass_kernel_spmd(_NC_CACHE["nc0"], in_maps, core_ids=list(range(8)))
        for b in range(8):
            in_maps[b]["x"] = np.ascontiguousarray(np.asarray(r0.results[b]["out"]).astype(np.float32))
        res = run_bass_kernel_spmd(_NC_CACHE["nc1"], in_maps, core_ids=list(range(8)))
    out = np.stack([np.asarray(r["out"]).astype(np.float32) for r in res.results], axis=0)
    return out.astype(inp["x"].dtype)
```
